# Optimizing a Trainium2 kernel written in Bass

```python
import jax
import jax.numpy as jnp
from jax import lax
import numpy as np

D_MODEL = 1024
BATCH = 8
SEQ = 8192
DEPTH = 2

CTX_LEN = 256
GRID_W = 64
ATTN_HEADS = 8
ATTN_KV_HEADS = 2
HEAD_DIM = 64
WINDOW = 128
WBLOCK = 128
ROPE_THETA = 10000.0
HG_HEADS = 4
HG_DK = 128
HG_DV = 128
HG_CHUNK = 64
N_EXPERTS = 16
EXPERT_FF = 1024
CAPACITY_FACTOR = 2
N_MOD = 6
LN_EPS = 1e-6
DEEPNORM_ALPHA = (2 * DEPTH) ** 0.25
DEEPNORM_BETA = (8 * DEPTH) ** -0.25
ATTN_Q_DIM = ATTN_HEADS * HEAD_DIM
ATTN_KV_DIM = ATTN_KV_HEADS * HEAD_DIM
HG_K_DIM = HG_HEADS * HG_DK
HG_V_DIM = HG_HEADS * HG_DV
IN_SIZES = (ATTN_Q_DIM, ATTN_KV_DIM, ATTN_KV_DIM, HG_K_DIM, HG_K_DIM, HG_K_DIM, HG_V_DIM, HG_V_DIM, D_MODEL, D_MODEL)
IN_DIM = sum(IN_SIZES)

kernel_name = 'hybrid_swa_hgrn2_ecmoe_diffusion_block'


def layer_norm(x):
    xf = x.astype(jnp.float32)
    mu = jnp.mean(xf, axis=-1, keepdims=True)
    var = jnp.mean(jnp.square(xf - mu), axis=-1, keepdims=True)
    return ((xf - mu) * lax.rsqrt(var + LN_EPS)).astype(x.dtype)


def post_norm(x, g, b):
    return layer_norm(x) * g + b


def axial_rope(x):
    n_tok = x.shape[1]
    rows = n_tok // GRID_W
    row = jnp.broadcast_to(jnp.arange(rows)[:, None], (rows, GRID_W)).reshape(-1)
    col = jnp.broadcast_to(jnp.arange(GRID_W)[None, :], (rows, GRID_W)).reshape(-1)
    n_freq = HEAD_DIM // 4
    inv_freq = ROPE_THETA ** (-jnp.arange(n_freq, dtype=jnp.float32) / n_freq)

    def rotate(xh, pos):
        ang = pos.astype(jnp.float32)[:, None] * inv_freq[None, :]
        cos = jnp.cos(ang)[None, :, None, :].astype(x.dtype)
        sin = jnp.sin(ang)[None, :, None, :].astype(x.dtype)
        x1, x2 = jnp.split(xh, 2, axis=-1)
        return jnp.concatenate([x1 * cos - x2 * sin, x2 * cos + x1 * sin], axis=-1)

    x_row, x_col = jnp.split(x, 2, axis=-1)
    return jnp.concatenate([rotate(x_row, row), rotate(x_col, col)], axis=-1)


def window_attention(q, k, v, kc, vc, sink):
    B, L, H, dh = q.shape
    G = k.shape[2]
    R = H // G
    nb = L // WBLOCK
    scale = dh ** -0.5
    qb = q.reshape(B, nb, WBLOCK, G, R, dh)

    def band(t):
        tb = t.reshape(B, nb, WBLOCK, G, dh)
        tp = jnp.pad(tb, ((0, 0), (1, 1), (0, 0), (0, 0), (0, 0)))
        return jnp.concatenate([tp[:, :-2], tp[:, 1:-1], tp[:, 2:]], axis=2)

    kw, vw = band(k), band(v)
    ipos = jnp.arange(nb)[:, None] * WBLOCK + jnp.arange(WBLOCK)[None, :]
    jpos = jnp.arange(nb)[:, None] * WBLOCK - WBLOCK + jnp.arange(3 * WBLOCK)[None, :]
    rel = jpos[:, None, :] - ipos[:, :, None]
    valid = (jnp.abs(rel) <= WINDOW) & (jpos[:, None, :] >= 0) & (jpos[:, None, :] < L)
    s_win = jnp.einsum('bntgrd,bnsgd->bngrts', qb, kw).astype(jnp.float32) * scale
    s_win = jnp.where(valid[None, :, None, None], s_win, -jnp.inf)
    s_ctx = jnp.einsum('bntgrd,bcgd->bngrtc', qb, kc).astype(jnp.float32) * scale
    s_sink = sink.astype(jnp.float32).reshape(1, 1, G, R, 1, 1)
    m = jnp.maximum(jnp.maximum(s_ctx.max(-1, keepdims=True), s_win.max(-1, keepdims=True)), s_sink)
    e_ctx = jnp.exp(s_ctx - m)
    e_win = jnp.exp(s_win - m)
    denom = jnp.exp(s_sink - m) + e_ctx.sum(-1, keepdims=True) + e_win.sum(-1, keepdims=True)
    o = jnp.einsum('bngrtc,bcgd->bngrtd', e_ctx, vc) + jnp.einsum('bngrts,bnsgd->bngrtd', e_win, vw)
    o = (o / denom).astype(v.dtype)
    return o.transpose(0, 1, 4, 2, 3, 5).reshape(B, L, H * dh)


def context_attention(q, k, v, sink):
    B, Lc, H, dh = q.shape
    G = k.shape[2]
    R = H // G
    qg = q.reshape(B, Lc, G, R, dh)
    s = jnp.einsum('btgrd,bsgd->bgrts', qg, k).astype(jnp.float32) * (dh ** -0.5)
    s_sink = jnp.broadcast_to(sink.astype(jnp.float32).reshape(1, G, R, 1, 1), s.shape[:-1] + (1,))
    p = jax.nn.softmax(jnp.concatenate([s_sink, s], axis=-1), axis=-1)
    o = jnp.einsum('bgrts,bsgd->btgrd', p[..., 1:].astype(v.dtype), v)
    return o.reshape(B, Lc, H * dh)


def hgrn_lower_bounds(logits):
    cum = jnp.cumsum(jax.nn.softmax(logits.astype(jnp.float32), axis=0), axis=0)
    return cum - cum[0:1]


def forget_gate(z, lb):
    zf = z.astype(jnp.float32)
    lbh = lb.reshape(HG_HEADS, HG_DK)
    log_f = jnp.logaddexp(jnp.log(lbh), jnp.log1p(-lbh) + jax.nn.log_sigmoid(zf))
    k = (1.0 - lbh) * jax.nn.sigmoid(-zf)
    return log_f, k


def hgrn_scan(q, k, v, log_f, s0):
    B, L, H, dk = q.shape
    dv = v.shape[-1]
    nc = L // HG_CHUNK

    def chunks(t):
        return t.reshape(B, nc, HG_CHUNK, H, t.shape[-1]).transpose(1, 0, 3, 2, 4)

    tri = jnp.tril(jnp.ones((HG_CHUNK, HG_CHUNK), dtype=bool))

    def step(S, inp):
        qc, kc, vc, gc = inp
        cum = jnp.cumsum(gc, axis=2)
        diff = cum[:, :, :, None, :] - cum[:, :, None, :, :]
        decay = jnp.exp(jnp.where(tri[:, :, None], diff, -jnp.inf))
        scores = jnp.einsum('bhtk,bhsk,bhtsk->bhts', qc.astype(jnp.float32), kc, decay)
        o = jnp.einsum('bhtk,bhkv->bhtv', qc * jnp.exp(cum), S) + jnp.einsum('bhts,bhsv->bhtv', scores, vc)
        tot = cum[:, :, -1:, :]
        S_new = jnp.exp(tot[:, :, 0, :, None]) * S + jnp.einsum('bhsk,bhsv->bhkv', kc * jnp.exp(tot - cum), vc)
        return S_new, o

    s_fin, o = lax.scan(step, s0, (chunks(q), chunks(k), chunks(v), chunks(log_f)))
    o = o.transpose(1, 0, 3, 2, 4).reshape(B, L, H, dv)
    return o, s_fin


def hgrn_bidir(q, v, z_fw, z_bw, qc, vc, zc_fw, zc_bw, lb_fw, lb_bw):
    B = q.shape[0]
    s0 = jnp.zeros((B, HG_HEADS, HG_DK, HG_DV), jnp.float32)
    flip = lambda t: jnp.flip(t, axis=1)
    log_f, k = forget_gate(zc_fw, lb_fw)
    oc_fw, s_fw = hgrn_scan(qc, k, vc, log_f, s0)
    log_f, k = forget_gate(z_fw, lb_fw)
    o_fw, _ = hgrn_scan(q, k, v, log_f, s_fw)
    log_f, k = forget_gate(flip(zc_bw), lb_bw)
    oc_bw, s_bw = hgrn_scan(flip(qc), k, flip(vc), log_f, s0)
    log_f, k = forget_gate(flip(z_bw), lb_bw)
    o_bw, _ = hgrn_scan(flip(q), k, flip(v), log_f, s_bw)
    return o_fw + flip(o_bw), oc_fw + flip(oc_bw)


def hgrn_readout(o, g, norm_g):
    B, L = o.shape[:2]
    of = o.astype(jnp.float32)
    of = of * lax.rsqrt(jnp.mean(jnp.square(of), axis=-1, keepdims=True) + LN_EPS)
    return of.reshape(B, L, HG_V_DIM).astype(g.dtype) * norm_g * jax.nn.silu(g)


def token_mixer(u, uc, w_in, sink, lb_fw, lb_bw, norm_g, w_br_a, w_br_h, w_out, need_ctx):
    B, L, _ = u.shape
    Lc = uc.shape[1]
    idx = np.cumsum(IN_SIZES)[:-1].tolist()
    qa, ka, va, qh, zf, zb, ih, gh, ga, gb = jnp.split(u @ w_in, idx, axis=-1)
    cqa, cka, cva, cqh, czf, czb, cih, cgh, cga, cgb = jnp.split(uc @ w_in, idx, axis=-1)

    def heads(t, h):
        return t.reshape(B, t.shape[1], h, -1)

    kc_a, vc_a = heads(cka, ATTN_KV_HEADS), heads(cva, ATTN_KV_HEADS)
    attn = window_attention(axial_rope(heads(qa, ATTN_HEADS)), axial_rope(heads(ka, ATTN_KV_HEADS)),
                            heads(va, ATTN_KV_HEADS), kc_a, vc_a, sink)
    o_h, oc_h = hgrn_bidir(heads(jax.nn.silu(qh), HG_HEADS), heads(ih, HG_HEADS),
                           heads(zf, HG_HEADS), heads(zb, HG_HEADS),
                           heads(jax.nn.silu(cqh), HG_HEADS), heads(cih, HG_HEADS),
                           heads(czf, HG_HEADS), heads(czb, HG_HEADS), lb_fw, lb_bw)
    hg = hgrn_readout(o_h, gh, norm_g)
    y = (jax.nn.sigmoid(ga) * (attn @ w_br_a) + jax.nn.sigmoid(gb) * (hg @ w_br_h)) @ w_out
    if not need_ctx:
        return y, None
    attn_c = context_attention(heads(cqa, ATTN_HEADS), kc_a, vc_a, sink)
    hg_c = hgrn_readout(oc_h, cgh, norm_g)
    yc = (jax.nn.sigmoid(cga) * (attn_c @ w_br_a) + jax.nn.sigmoid(cgb) * (hg_c @ w_br_h)) @ w_out
    return y, yc


def expert_choice_ffn(u, w_router, w_gate, w_up, w_down):
    B, n, D = u.shape
    cap = CAPACITY_FACTOR * n // N_EXPERTS
    aff = jax.nn.softmax((u @ w_router).astype(jnp.float32), axis=-1)
    top_w, top_idx = lax.top_k(jnp.swapaxes(aff, 1, 2), cap)
    xs = jax.vmap(lambda ub, ib: ub[ib])(u, top_idx)
    h = jax.nn.silu(jnp.einsum('becd,edf->becf', xs, w_gate)) * jnp.einsum('becd,edf->becf', xs, w_up)
    y = jnp.einsum('becf,efd->becd', h, w_down) * top_w[..., None].astype(u.dtype)
    return jax.vmap(lambda yb, ib: jnp.zeros((n, D), yb.dtype).at[ib.reshape(-1)].add(yb.reshape(-1, D)))(y, top_idx)


def setup_inputs(seed: int = 0) -> dict:
    key = jax.random.key(seed)
    ks = jax.random.split(key, 24)
    f32 = jnp.float32

    def nrm(k, shape, scale):
        return jax.random.normal(k, shape, f32) * scale

    D = D_MODEL
    return {
        'x': nrm(ks[0], (BATCH, SEQ, D), 1.0),
        'c': nrm(ks[1], (BATCH, D), 1.0),
        'ctx': nrm(ks[2], (BATCH, CTX_LEN, D), 1.0),
        'c_ctx': nrm(ks[3], (D,), 1.0),
        'w_mod': nrm(ks[4], (DEPTH, D, N_MOD * D), D ** -0.5),
        'b_mod': nrm(ks[5], (DEPTH, N_MOD * D), 0.02),
        'w_in': nrm(ks[6], (DEPTH, D, IN_DIM), D ** -0.5),
        'attn_sink': nrm(ks[7], (DEPTH, ATTN_HEADS), 1.0),
        'hgrn_lb_fw': nrm(ks[8], (DEPTH, HG_K_DIM), 0.5),
        'hgrn_lb_bw': nrm(ks[9], (DEPTH, HG_K_DIM), 0.5),
        'hgrn_norm_g': 1.0 + nrm(ks[10], (DEPTH, HG_V_DIM), 0.02),
        'w_branch_attn': nrm(ks[11], (DEPTH, ATTN_Q_DIM, D), DEEPNORM_BETA * ATTN_Q_DIM ** -0.5),
        'w_branch_hgrn': nrm(ks[12], (DEPTH, HG_V_DIM, D), DEEPNORM_BETA * HG_V_DIM ** -0.5),
        'w_out': nrm(ks[13], (DEPTH, D, D), DEEPNORM_BETA * D ** -0.5),
        'w_router': nrm(ks[14], (DEPTH, D, N_EXPERTS), D ** -0.5),
        'w_gate': nrm(ks[15], (DEPTH, N_EXPERTS, D, EXPERT_FF), D ** -0.5),
        'w_up': nrm(ks[16], (DEPTH, N_EXPERTS, D, EXPERT_FF), D ** -0.5),
        'w_down': nrm(ks[17], (DEPTH, N_EXPERTS, EXPERT_FF, D), DEEPNORM_BETA * EXPERT_FF ** -0.5),
        'ln_g': 1.0 + nrm(ks[18], (DEPTH, 2, D), 0.02),
        'ln_b': nrm(ks[19], (DEPTH, 2, D), 0.02),
    }


def reference(x, c, ctx, c_ctx, w_mod, b_mod, w_in, attn_sink, hgrn_lb_fw, hgrn_lb_bw, hgrn_norm_g,
              w_branch_attn, w_branch_hgrn, w_out, w_router, w_gate, w_up, w_down, ln_g, ln_b):
    lb_fw_all = hgrn_lower_bounds(hgrn_lb_fw)
    lb_bw_all = hgrn_lower_bounds(hgrn_lb_bw)
    xc = ctx
    for l in range(DEPTH):
        need_ctx = l < DEPTH - 1
        mod = jax.nn.silu(c) @ w_mod[l] + b_mod[l]
        modc = jax.nn.silu(c_ctx) @ w_mod[l] + b_mod[l]
        sh1, sc1, g1, sh2, sc2, g2 = jnp.split(mod[:, None, :], N_MOD, axis=-1)
        sh1c, sc1c, g1c, sh2c, sc2c, g2c = jnp.split(modc, N_MOD)
        u = layer_norm(x) * (1.0 + sc1) + sh1
        uc = layer_norm(xc) * (1.0 + sc1c) + sh1c
        y, yc = token_mixer(u, uc, w_in[l], attn_sink[l], lb_fw_all[l], lb_bw_all[l], hgrn_norm_g[l],
                            w_branch_attn[l], w_branch_hgrn[l], w_out[l], need_ctx)
        x = post_norm(DEEPNORM_ALPHA * x + g1 * y, ln_g[l, 0], ln_b[l, 0])
        u = layer_norm(x) * (1.0 + sc2) + sh2
        x = post_norm(DEEPNORM_ALPHA * x + g2 * expert_choice_ffn(u, w_router[l], w_gate[l], w_up[l], w_down[l]),
                      ln_g[l, 1], ln_b[l, 1])
        if need_ctx:
            xc = post_norm(DEEPNORM_ALPHA * xc + g1c * yc, ln_g[l, 0], ln_b[l, 0])
            uc = layer_norm(xc) * (1.0 + sc2c) + sh2c
            xc = post_norm(DEEPNORM_ALPHA * xc + g2c * expert_choice_ffn(uc, w_router[l], w_gate[l], w_up[l], w_down[l]),
                           ln_g[l, 1], ln_b[l, 1])
    return x
```

```python
import numpy as np
import ml_dtypes
from contextlib import ExitStack
import concourse.bass as bass
import concourse.mybir as mybir
from concourse.bass_utils import run_bass_kernel_spmd

F32 = mybir.dt.float32
BF16 = mybir.dt.bfloat16
I32 = mybir.dt.int32
U32 = mybir.dt.uint32
ALU = mybir.AluOpType
AF = mybir.ActivationFunctionType
AX = mybir.AxisListType

D = 1024
LCTX = 256
LSEQ = 8192
NT = (LCTX + LSEQ) // 128
NTOK = NT * 128
IN_DIM = 5376
NEXP = 16
CAP = 1024
CAPC = 32
LROW = CAP + CAPC
LN_EPS = 1e-6
ALPHA = 4.0 ** 0.25


class Buf:
    def __init__(self, kb, t, name):
        self.kb = kb
        self.t = t
        self.name = name
        self.w = None
        self.r = {}
        self.dr = 0
        self.dsem = None
        self.dcnt = 0

    def __getitem__(self, k):
        return self.t[k]

    def sem(self):
        if self.dsem is None:
            if self.kb.free_sems:
                self.dsem, self.dcnt = self.kb.free_sems.pop()
            else:
                self.dsem = self.kb.new_sem(self.name)
                self.dcnt = 0
        return self.dsem


class KB:
    ENG = ('pe', 'act', 'dve', 'pool', 'sp')

    def __init__(self):
        self.nc = bass.Bass("TRN2", target_bir_lowering=False)
        self.es = ExitStack()
        self.q = {e: [] for e in self.ENG}
        self.cnt = {e: 0 for e in self.ENG}
        self.pend = {e: False for e in self.ENG}
        self.known = {e: {} for e in self.ENG}
        self.nsem = 0
        self.psem = {e: self.new_sem('prog_' + e) for e in self.ENG}
        self.nbuf = 0
        self.relax_self = True
        self.inflight = {}
        self.free_sems = []
        self.stage_bufs = []

    def new_sem(self, name):
        self.nsem += 1
        return self.es.enter_context(self.nc.semaphore('s%d_%s' % (self.nsem, name)))

    def sb(self, shape, dt, name=None, stack=None):
        self.nbuf += 1
        name = '%s_%d' % (name or 'sb', self.nbuf)
        t = (stack or self.es).enter_context(self.nc.sbuf_tensor(name, list(shape), dt))
        b = Buf(self, t, name)
        if stack is not None:
            self.stage_bufs.append(b)
        return b

    def ps(self, shape, dt, name=None, stack=None):
        self.nbuf += 1
        name = '%s_%d' % (name or 'ps', self.nbuf)
        t = (stack or self.es).enter_context(self.nc.psum_tensor(name, list(shape), dt))
        b = Buf(self, t, name)
        if stack is not None:
            self.stage_bufs.append(b)
        return b

    def dram(self, name, shape, dt, kind="Internal"):
        return self.nc.dram_tensor(name, list(shape), dt, kind=kind)

    def _wait(self, eng, dep):
        if dep is None:
            return
        if dep[0] == 'e':
            _, e2, n = dep
            if e2 == 'pe' and eng == 'pe':
                return
            if e2 == eng and self.relax_self and n < self.cnt[eng]:
                return
            if self.known[eng].get(e2, 0) >= n:
                return
            self.known[eng][e2] = n
            self.q[eng].append(('w', self.psem[e2], n))
        else:
            _, sem, n = dep
            key = ('d', id(sem))
            if self.known[eng].get(key, 0) >= n:
                return
            self.known[eng][key] = n
            self.q[eng].append(('w', sem, n))

    def _deps(self, eng, reads, writes):
        for b in reads:
            self._wait(eng, b.w)
        for b in writes:
            self._wait(eng, b.w)
            for e2, n in b.r.items():
                if e2 != eng:
                    self._wait(eng, ('e', e2, n))
            if b.dr:
                self._wait(eng, ('d', b.sem(), b.dr))

    def op(self, eng, meth, reads=(), writes=(), inc=True, **kw):
        fn = (lambda e, m=meth, k=kw: getattr(e, m)(**k))
        self._deps(eng, reads, writes)
        n = self.cnt[eng] + 1
        if inc:
            self.cnt[eng] = n
            self.q[eng].append(('i', fn, self.psem[eng]))
            self.pend[eng] = False
        else:
            self.q[eng].append(('i', fn, None))
            self.pend[eng] = True
        for b in reads:
            b.r[eng] = n
        for b in writes:
            b.w = ('e', eng, n)
            b.r = {}
            b.dr = 0

    def dma(self, eng, out_ap, in_ap, reads=(), writes=(), fn=None):
        self._deps(eng, reads, writes)
        assert len(reads) + len(writes) >= 1
        tgt = (list(writes) + list(reads))[0]
        sem = tgt.sem()
        tgt.dcnt += 16
        val = tgt.dcnt
        if fn is None:
            fn = lambda e, o=out_ap, i=in_ap: e.dma_start(out=o, in_=i)
        self.q[eng].append(('d', fn, sem))
        self.inflight[id(sem)] = (sem, val)
        for b in writes:
            b.w = ('d', sem, val)
            b.r = {}
            b.dr = 0
        for b in reads:
            assert b is tgt or b.dsem is None or b.dsem is sem
            b.dsem = sem
            b.dcnt = max(b.dcnt, val)
            b.dr = val
        return sem, val

    def wait_dma(self, eng, sem, val):
        self._wait(eng, ('d', sem, val))

    def stage_end(self):
        for sem, val in self.inflight.values():
            self._wait('sp', ('d', sem, val))
        self.inflight = {}
        for e2 in self.ENG:
            if e2 != 'sp':
                self._wait('sp', ('e', e2, self.cnt[e2]))
        self.cnt['sp'] += 1
        self.q['sp'].append(('s', self.psem['sp']))
        for e in self.ENG:
            for e2 in self.ENG:
                if e2 != e:
                    self._wait(e, ('e', e2, self.cnt[e2]))
        self.flush()
        for b in self.stage_bufs:
            if b.dsem is not None:
                self.free_sems.append((b.dsem, b.dcnt))
                b.dsem = None
        self.stage_bufs = []

    def flush(self):
        nc = self.nc
        q = self.q
        with nc.Block() as block:
            def run(e, lst):
                for it in lst:
                    if it[0] == 'w':
                        e.wait_ge(it[1], it[2])
                    elif it[0] == 'i':
                        ins = it[1](e)
                        if it[2] is not None:
                            ins.then_inc(it[2], 1)
                    elif it[0] == 's':
                        e.sem_inc(it[1], 1)
                    else:
                        it[1](e).then_inc(it[2], 16)

            @block.tensor
            def _(e):
                run(e, q['pe'])

            @block.scalar
            def _(e):
                run(e, q['act'])

            @block.vector
            def _(e):
                run(e, q['dve'])

            @block.gpsimd
            def _(e):
                run(e, q['pool'])

            @block.sync
            def _(e):
                run(e, q['sp'])
        self.q = {e: [] for e in self.ENG}


class Pool:
    def __init__(self, bufs):
        self.bufs = bufs
        self.i = 0

    def get(self):
        b = self.bufs[self.i % len(self.bufs)]
        self.i += 1
        return b


C_ID, C_TIF, C_TIB, C_TSF, C_TSB, C_BD, C_OFF, C_IOTA, C_SEGB, C_EB, C_ONES, C_W = \
    0, 128, 256, 384, 512, 640, 768, 896, 1152, 1153, 1154, 1282


def make_consts():
    a = np.arange(128)[:, None]
    b = np.arange(128)[None, :]
    cp = np.zeros((128, C_W), np.float32)
    cp[:, C_ID:C_ID + 128] = (a == b)
    cp[:, C_TIF:C_TIF + 128] = (a <= b)
    cp[:, C_TIB:C_TIB + 128] = (a >= b)
    cp[:, C_TSF:C_TSF + 128] = (a > b)
    cp[:, C_TSB:C_TSB + 128] = (a < b)
    cp[:, C_BD:C_BD + 128] = (a // 8 == b // 8)
    cp[:, C_OFF:C_OFF + 128] = (a // 8 == b // 8) & (a % 8 < b % 8)
    cp[:, C_IOTA:C_IOTA + 256] = np.arange(256)[None, :]
    cp[:, C_SEGB] = (np.arange(128) % 8) * 1024 + LCTX
    cp[:, C_EB] = (np.arange(128) // 8) * LROW
    cp[:, C_ONES:C_ONES + 128] = 1.0
    rope = np.zeros((NTOK, 64), np.float32)
    rope[:, :32] = 1.0
    t = np.arange(LSEQ)
    row = (t // 64).astype(np.float32)
    col = (t % 64).astype(np.float32)
    inv = (10000.0 ** (-np.arange(16, dtype=np.float32) / 16)).astype(np.float32)
    ar = row[:, None] * inv[None, :]
    ac = col[:, None] * inv[None, :]
    rope[LCTX:, 0:16] = np.cos(ar)
    rope[LCTX:, 16:32] = np.cos(ac)
    rope[LCTX:, 32:48] = np.sin(ar)
    rope[LCTX:, 48:64] = np.sin(ac)
    return cp, rope


def groups():
    g = [(0, 2)]
    for s in range(2, NT, 4):
        g.append((s, 4))
    return g


class Prog:
    def __init__(self, debug=False):
        self.kb = kb = KB()
        self.debug = debug
        E = "ExternalInput"
        d = kb.dram
        self.x_all = d("x_all", [NTOK, D], F32, E)
        self.c2 = d("c2", [16, 128], F32, E)
        self.cpack = d("cpack", [128, C_W], F32, E)
        self.rope = d("rope", [NTOK, 64], F32, E)
        self.w_mod = d("w_mod", [2, D, 6 * D], F32, E)
        self.b_mod = d("b_mod", [2, 6 * D], F32, E)
        self.w_in = d("w_in", [2, D, IN_DIM], F32, E)
        self.attn_sink = d("attn_sink", [2, 8], F32, E)
        self.lb_fw = d("hgrn_lb_fw", [2, 512], F32, E)
        self.lb_bw = d("hgrn_lb_bw", [2, 512], F32, E)
        self.norm_g = d("hgrn_norm_g", [2, 512], F32, E)
        self.w_ba = d("w_branch_attn", [2, 512, D], F32, E)
        self.w_bh = d("w_branch_hgrn", [2, 512, D], F32, E)
        self.w_out = d("w_out", [2, D, D], F32, E)
        self.w_router = d("w_router", [2, D, NEXP], F32, E)
        self.w_gate = d("w_gate", [2, NEXP, D, D], F32, E)
        self.w_up = d("w_up", [2, NEXP, D, D], F32, E)
        self.w_down = d("w_down", [2, NEXP, D, D], F32, E)
        self.ln_g = d("ln_g", [2, 2, D], F32, E)
        self.ln_b = d("ln_b", [2, 2, D], F32, E)
        self.y = d("y", [LSEQ, D], F32, "ExternalOutput")
        dbg = debug
        self.modrow = d("modrow", [2, 6 * D], F32, "ExternalOutput" if (dbg is True or (dbg and "modrow" in dbg)) else "Internal")
        self.qT = d("s_qT", [512, NTOK], BF16, "ExternalOutput" if (dbg is True or (dbg and "s_qT" in dbg)) else "Internal")
        self.kT = d("s_kT", [128, NTOK], BF16, "ExternalOutput" if (dbg is True or (dbg and "s_kT" in dbg)) else "Internal")
        self.va = d("s_va", [NTOK, 128], BF16, "ExternalOutput" if (dbg is True or (dbg and "s_va" in dbg)) else "Internal")
        self.qhT = d("s_qhT", [512, NTOK], BF16, "ExternalOutput" if (dbg is True or (dbg and "s_qhT" in dbg)) else "Internal")
        self.lf = d("s_lf", [NTOK, 1024], F32, "ExternalOutput" if (dbg is True or (dbg and "s_lf" in dbg)) else "Internal")
        self.kk = d("s_kk", [NTOK, 1024], BF16, "ExternalOutput" if (dbg is True or (dbg and "s_kk" in dbg)) else "Internal")
        self.kkT = d("s_kkT", [1024, NTOK], BF16, "ExternalOutput" if (dbg is True or (dbg and "s_kkT" in dbg)) else "Internal")
        self.ih = d("s_ih", [NTOK, 512], BF16, "ExternalOutput" if (dbg is True or (dbg and "s_ih" in dbg)) else "Internal")
        self.sgh = d("s_sgh", [NTOK, 512], BF16, "ExternalOutput" if (dbg is True or (dbg and "s_sgh" in dbg)) else "Internal")
        self.sgaT = d("s_sgaT", [1024, NTOK], BF16, "ExternalOutput" if (dbg is True or (dbg and "s_sgaT" in dbg)) else "Internal")
        self.sgbT = d("s_sgbT", [1024, NTOK], BF16, "ExternalOutput" if (dbg is True or (dbg and "s_sgbT" in dbg)) else "Internal")
        self.ofw = d("s_ofw", [NTOK, 512], F32, "ExternalOutput" if (dbg is True or (dbg and "s_ofw" in dbg)) else "Internal")
        self.hgT = d("s_hgT", [512, NTOK], BF16, "ExternalOutput" if (dbg is True or (dbg and "s_hgT" in dbg)) else "Internal")
        self.attnT = d("s_attnT", [512, NTOK], BF16, "ExternalOutput" if (dbg is True or (dbg and "s_attnT" in dbg)) else "Internal")
        self.x1 = d("s_x1", [NTOK, D], F32, "ExternalOutput" if (dbg is True or (dbg and "s_x1" in dbg)) else "Internal")
        self.u2 = d("s_u2", [NTOK, D], BF16, "ExternalOutput" if (dbg is True or (dbg and "s_u2" in dbg)) else "Internal")
        self.affT = d("s_affT", [NEXP, NTOK], F32, "ExternalOutput" if (dbg is True or (dbg and "s_affT" in dbg)) else "Internal")
        self.pairs = d("s_pairs", [NEXP * LROW + 256, 2], F32, "ExternalOutput" if (dbg is True or (dbg and "s_pairs" in dbg)) else "Internal")
        self.acc = d("s_acc", [NTOK, D], F32, "ExternalOutput" if (dbg is True or (dbg and "s_acc" in dbg)) else "Internal")
        self.xa = d("s_xa", [NTOK, D], F32, "ExternalOutput" if (dbg is True or (dbg and "s_xa" in dbg)) else "Internal")
        self.cp = kb.sb([128, C_W], F32, 'cp')
        self.idb = kb.sb([128, 128], BF16, 'idb')
        self.mfw = kb.sb([128, 128], BF16, 'mfw')
        self.mbw = kb.sb([128, 128], BF16, 'mbw')
        kb.dma('sp', self.cp[:], self.cpack.ap(), writes=[self.cp])
        kb.op('dve', 'tensor_copy', out=self.idb[:], in_=self.cp[:, C_ID:C_ID + 128], reads=[self.cp], writes=[self.idb])
        kb.op('dve', 'tensor_copy', out=self.mfw[:], in_=self.cp[:, C_TIF:C_TIF + 128], reads=[self.cp], writes=[self.mfw])
        kb.op('dve', 'tensor_copy', out=self.mbw[:], in_=self.cp[:, C_TIB:C_TIB + 128], reads=[self.cp], writes=[self.mbw])
        self.ngp = kb.sb([128, 128], BF16, 'ngp')
        self.ngn = kb.sb([128, 128], BF16, 'ngn')
        kb.op('dve', 'tensor_scalar', out=self.ngp[:], in0=self.cp[:, C_TIB:C_TIB + 128], scalar1=30000.0, scalar2=-30000.0,
              op0=ALU.mult, op1=ALU.add, reads=[self.cp], writes=[self.ngp])
        kb.op('dve', 'tensor_scalar', out=self.ngn[:], in0=self.cp[:, C_TIF:C_TIF + 128], scalar1=30000.0, scalar2=-30000.0,
              op0=ALU.mult, op1=ALU.add, reads=[self.cp], writes=[self.ngn])
        kb.stage_end()

    def breg(self, eng, val):
        if not hasattr(self, '_bregs'):
            self._bregs = {}
        if val not in self._bregs:
            self._bregs[val] = eng.to_reg(val)
        return self._bregs[val]

    def ln_stats(self, st, src, rs, nm, stt, mv):
        kb = self.kb
        kb.op('dve', 'bn_stats', out=stt[:, 0, :], in_=src[:, 0:512], reads=[src], writes=[stt])
        kb.op('dve', 'bn_stats', out=stt[:, 1, :], in_=src[:, 512:1024], reads=[src], writes=[stt])
        kb.op('dve', 'bn_aggr', out=mv[:], in_=stt[:].rearrange("p a b -> p (a b)"), reads=[stt], writes=[mv])
        kb.op('act', 'activation', out=rs[:], in_=mv[:, 1:2], func=AF.Sqrt, bias=LN_EPS, scale=1.0,
              reads=[mv], writes=[rs])
        kb.op('dve', 'reciprocal', out=rs[:], in_=rs[:], reads=[rs], writes=[rs])
        kb.op('pool', 'tensor_scalar', out=nm[:], in0=mv[:, 0:1], scalar1=rs[:], scalar2=-1.0,
              op0=ALU.mult, op1=ALU.mult, reads=[mv, rs], writes=[nm])

    def bcast_row(self, st, dram_ap_1d, n, name):
        kb = self.kb
        t = kb.sb([128, n], F32, name, stack=st)
        kb.dma('sp', t[:], dram_ap_1d.partition_broadcast(128), writes=[t])
        return t

    def mk_eps(self, st):
        kb = self.kb
        self.eps = kb.sb([128, 1], F32, 'eps', stack=st)
        kb.op('pool', 'memset', ap=self.eps[:], constant=LN_EPS, writes=[self.eps])

    def stage0(self, l):
        kb = self.kb
        with ExitStack() as st:
            c2s = kb.sb([16, 128], F32, 'c2s', stack=st)
            kb.dma('sp', c2s[:], self.c2.ap(), writes=[c2s])
            tp = kb.ps([128, 16], F32, 'tp0', stack=st)
            kb.op('pe', 'transpose', out=tp[:], in_=c2s[:], identity=self.cp[0:16, C_ID:C_ID + 16], reads=[c2s, self.cp], writes=[tp])
            sil = kb.sb([128, 2, 8], F32, 'sil', stack=st)
            kb.op('act', 'activation', out=sil[:].rearrange("p a b -> p (a b)"), in_=tp[:], func=AF.Silu, reads=[tp], writes=[sil])
            bm = kb.sb([2, 6 * D], F32, 'bm', stack=st)
            kb.dma('sp', bm[:], self.b_mod.ap()[l].partition_broadcast(2), writes=[bm])
            mod = kb.sb([2, 6 * D], F32, 'mod', stack=st)
            wms = Pool([kb.sb([128, 8, 512], F32, 'wm', stack=st) for _ in range(2)])
            pps = Pool([kb.ps([2, 512], F32, 'pm', stack=st) for _ in range(2)])
            for n in range(12):
                wm = wms.get()
                kb.dma('sp', wm[:], self.w_mod.ap()[l][:, n * 512:(n + 1) * 512].rearrange("(kc k) n -> k kc n", k=128), writes=[wm])
                pm = pps.get()
                for kc in range(8):
                    kb.op('pe', 'matmul', out=pm[:], lhsT=sil[:, :, kc], rhs=wm[:, kc, :], start=(kc == 0), stop=(kc == 7),
                          reads=[sil, wm], writes=[pm], inc=(kc == 7))
                kb.op('dve', 'tensor_tensor', out=mod[:, n * 512:(n + 1) * 512], in0=pm[:], in1=bm[:, n * 512:(n + 1) * 512], op=ALU.add,
                      reads=[pm, bm], writes=[mod])
            for c0 in (1024, 4096):
                kb.op('dve', 'tensor_scalar_add', out=mod[:, c0:c0 + 1024], in0=mod[:, c0:c0 + 1024], scalar1=1.0, reads=[mod], writes=[mod])
            kb.dma('sp', self.modrow.ap(), mod[:], reads=[mod])
            kb.stage_end()

    def modtile(self, st, r, k, name):
        return self.bcast_row(st, self.modrow.ap()[r, k * D:(k + 1) * D], D, name)

    def stageA(self, l, xin):
        kb = self.kb
        cp = self.cp
        with ExitStack() as st:
            sb = lambda shape, dt, name: kb.sb(shape, dt, name, stack=st)
            W = sb([128, 8, IN_DIM], BF16, 'W')
            wst = Pool([sb([128, 8, 256], F32, 'wst') for _ in range(2)])
            for n in range(IN_DIM // 256):
                ws = wst.get()
                kb.dma('sp', ws[:], self.w_in.ap()[l][:, n * 256:(n + 1) * 256].rearrange("(kc k) n -> k kc n", k=128), writes=[ws])
                kb.op('dve' if n % 2 else 'act', 'tensor_copy' if n % 2 else 'copy', out=W[:, :, n * 256:(n + 1) * 256], in_=ws[:], reads=[ws], writes=[W])
            A1 = [self.modtile(st, r, 1, 'A1') for r in range(2)]
            B1 = [self.modtile(st, r, 0, 'B1') for r in range(2)]
            if l == 1:
                lbl = sb([128, 2, 2, 512], F32, 'lbl')
                kb.dma('sp', lbl[:, 0], self.lb_fw.ap().partition_broadcast(128), writes=[lbl])
                kb.dma('sp', lbl[:, 1], self.lb_bw.ap().partition_broadcast(128), writes=[lbl])
                LB = sb([128, 2, 512], F32, 'LB')
                OML = sb([128, 2, 512], F32, 'OML')
                kb.op('dve', 'tensor_tensor', out=LB[:], in0=lbl[:, :, 1, :], in1=lbl[:, :, 0, :], op=ALU.subtract, reads=[lbl], writes=[LB])
                kb.op('act', 'activation', out=LB[:], in_=LB[:], func=AF.Sigmoid, reads=[LB], writes=[LB])
                kb.op('dve', 'tensor_scalar', out=OML[:], in0=LB[:], scalar1=-1.0, scalar2=1.0, op0=ALU.mult, op1=ALU.add, reads=[LB], writes=[OML])
            xts = Pool([sb([128, D], F32, 'xt') for _ in range(2)])
            xns = Pool([sb([128, D], F32, 'xn') for _ in range(1)])
            us = Pool([sb([128, D], BF16, 'u') for _ in range(1)])
            uTs = Pool([sb([128, 8, 512], BF16, 'uT') for _ in range(2)])
            stt = Pool([sb([128, 2, 6], F32, 'stt') for _ in range(2)])
            mvs = Pool([sb([128, 2], F32, 'mv') for _ in range(2)])
            rss = Pool([sb([128, 1], F32, 'rs') for _ in range(2)])
            nms = Pool([sb([128, 1], F32, 'nm') for _ in range(2)])
            rts = Pool([sb([128, 64], F32, 'rt') for _ in range(2)])
            fos = Pool([sb([128, 512], BF16, 'fo') for _ in range(3)])
            qrs = Pool([sb([128, 640], BF16, 'qr') for _ in range(2)])
            tmpq = [sb([128, 256], F32, 'tmpq%d' % i) for i in range(4)]
            tmpk = [sb([128, 64], F32, 'tmpk%d' % i) for i in range(4)]
            ksbs = Pool([sb([128, 128], F32, 'ksb') for _ in range(2)])
            qkTs = Pool([sb([128, 5, 128], BF16, 'qkT') for _ in range(2)])
            vas = Pool([sb([128, 128], BF16, 'va') for _ in range(2)])
            sgs = Pool([sb([128, 1024], F32, 'sg') for _ in range(1)])
            lfs = Pool([sb([128, 1024], F32, 'lfo') for _ in range(1)])
            kks = Pool([sb([128, 1024], BF16, 'kko') for _ in range(2)])
            kkTs = Pool([sb([128, 8, 128], BF16, 'kkTo') for _ in range(2)])
            ihs = Pool([sb([128, 512], BF16, 'iho') for _ in range(2)])
            ghs = Pool([sb([128, 512], BF16, 'gho') for _ in range(2)])
            pp = Pool([kb.ps([128, 512], F32, 'ppA', stack=st) for _ in range(8)])

            def proj_tok(uT, j, c0, ncols):
                p = pp.get()
                for kc in range(8):
                    kb.op('pe', 'matmul', out=p[:, 0:ncols], lhsT=uT[:, kc, j * 128:(j + 1) * 128], rhs=W[:, kc, c0:c0 + ncols],
                          start=(kc == 0), stop=(kc == 7), reads=[uT, W], writes=[p], inc=(kc == 7))
                return p

            def tr_bf(src_ap, src_buf, nblk):
                p = pp.get()
                pv = p[:].bitcast(BF16).rearrange("p (a b) -> p a b", b=128)
                for k in range(nblk):
                    kb.op('pe', 'transpose', out=pv[:, k, :], in_=src_ap[:, k * 128:(k + 1) * 128], identity=self.idb[:],
                          reads=[src_buf, self.idb], writes=[p], inc=(k == nblk - 1))
                return p, pv

            for (t0, nt) in groups():
                N = nt * 128
                r = 1 if t0 < 2 else 0
                uT = uTs.get()
                for j in range(nt):
                    ti = t0 + j
                    xt = xts.get()
                    kb.dma('sp', xt[:], xin.ap()[ti * 128:(ti + 1) * 128, :], writes=[xt])
                    s6, mv, rs, nm = stt.get(), mvs.get(), rss.get(), nms.get()
                    self.ln_stats(st, xt, rs, nm, s6, mv)
                    xn = xns.get()
                    kb.op('act', 'activation', out=xn[:], in_=xt[:], func=AF.Identity, bias=nm[:], scale=rs[:], reads=[xt, nm, rs], writes=[xn])
                    kb.op('dve', 'tensor_tensor', out=xn[:], in0=xn[:], in1=A1[r][:], op=ALU.mult, reads=[xn, A1[r]], writes=[xn])
                    u = us.get()
                    kb.op('dve', 'tensor_tensor', out=u[:], in0=xn[:], in1=B1[r][:], op=ALU.add, reads=[xn, B1[r]], writes=[u])
                    p, pv = tr_bf(u[:], u, 8)
                    kb.op('act', 'copy', out=uT[:, :, j * 128:(j + 1) * 128], in_=pv[:, 0:8, :], reads=[p], writes=[uT])
                for (c0, nch, dst, fn) in ((768, 4, self.qhT, AF.Silu), (3328, 8, self.sgaT, AF.Sigmoid), (4352, 8, self.sgbT, AF.Sigmoid)):
                    for n in range(nch):
                        p = pp.get()
                        for kc in range(8):
                            kb.op('pe', 'matmul', out=p[:, 0:N], lhsT=W[:, kc, c0 + n * 128:c0 + (n + 1) * 128], rhs=uT[:, kc, 0:N],
                                  start=(kc == 0), stop=(kc == 7), reads=[uT, W], writes=[p], inc=(kc == 7))
                        fo = fos.get()
                        kb.op('act', 'activation', out=fo[:, 0:N], in_=p[:, 0:N], func=fn, reads=[p], writes=[fo])
                        kb.dma('pool', dst.ap()[n * 128:(n + 1) * 128, t0 * 128:t0 * 128 + N], fo[:, 0:N], reads=[fo])
                for j in range(nt):
                    ti = t0 + j
                    tok = slice(ti * 128, (ti + 1) * 128)
                    rt = rts.get()
                    kb.dma('sp', rt[:], self.rope.ap()[tok, :], writes=[rt])
                    pq = proj_tok(uT, j, 0, 512)
                    pk = proj_tok(uT, j, 512, 256)
                    qr = qrs.get()
                    ksb = ksbs.get()
                    kb.op('act', 'copy', out=ksb[:], in_=pk[:, 0:128], reads=[pk], writes=[ksb])
                    for (src, H, o0, eng, tmp) in ((pq, 8, 0, 'dve', tmpq), (ksb, 2, 512, 'pool', tmpk)):
                        sv = src[:, 0:H * 64].rearrange("p (h a b c) -> p h a b c", h=H, a=2, b=2)
                        x1, x2 = sv[:, :, :, 0, :], sv[:, :, :, 1, :]
                        cosb = rt[:, 0:32].rearrange("p (a c) -> p a c", a=2).unsqueeze(1).to_broadcast([128, H, 2, 16])
                        sinb = rt[:, 32:64].rearrange("p (a c) -> p a c", a=2).unsqueeze(1).to_broadcast([128, H, 2, 16])
                        tv = [t[:, 0:H * 32].rearrange("p (h a c) -> p h a c", h=H, a=2) for t in tmp]
                        ov = qr[:, o0:o0 + H * 64].rearrange("p (h a b c) -> p h a b c", h=H, a=2, b=2)
                        kb.op(eng, 'tensor_tensor', out=tv[0], in0=x1, in1=cosb, op=ALU.mult, reads=[src, rt], writes=[tmp[0]])
                        kb.op(eng, 'tensor_tensor', out=tv[1], in0=x2, in1=sinb, op=ALU.mult, reads=[src, rt], writes=[tmp[1]])
                        kb.op(eng, 'tensor_tensor', out=tv[2], in0=x2, in1=cosb, op=ALU.mult, reads=[src, rt], writes=[tmp[2]])
                        kb.op(eng, 'tensor_tensor', out=tv[3], in0=x1, in1=sinb, op=ALU.mult, reads=[src, rt], writes=[tmp[3]])
                        kb.op(eng, 'tensor_tensor', out=ov[:, :, :, 0, :], in0=tv[0], in1=tv[1], op=ALU.subtract, reads=[tmp[0], tmp[1]], writes=[qr])
                        kb.op(eng, 'tensor_tensor', out=ov[:, :, :, 1, :], in0=tv[2], in1=tv[3], op=ALU.add, reads=[tmp[2], tmp[3]], writes=[qr])
                    vo = vas.get()
                    kb.op('act', 'copy', out=vo[:], in_=pk[:, 128:256], reads=[pk], writes=[vo])
                    kb.dma('pool', self.va.ap()[tok, :], vo[:], reads=[vo])
                    p, pv = tr_bf(qr[:], qr, 5)
                    qkT = qkTs.get()
                    kb.op('act', 'copy', out=qkT[:], in_=pv[:, 0:5, :], reads=[p], writes=[qkT])
                    kb.dma('pool', self.qT.ap()[:, tok].rearrange("(j p) t -> p j t", p=128), qkT[:, 0:4, :], reads=[qkT])
                    kb.dma('pool', self.kT.ap()[:, tok], qkT[:, 4, :], reads=[qkT])
                    pzf = proj_tok(uT, j, 1280, 512)
                    pzb = proj_tok(uT, j, 1792, 512)
                    sg = sgs.get()
                    kb.op('act', 'activation', out=sg[:, 0:512], in_=pzf[:], func=AF.Sigmoid, reads=[pzf], writes=[sg])
                    kb.op('act', 'activation', out=sg[:, 512:1024], in_=pzb[:], func=AF.Sigmoid, reads=[pzb], writes=[sg])
                    if l == 1:
                        kb.op('dve', 'tensor_tensor', out=sg[:], in0=sg[:], in1=OML[:].rearrange("p a b -> p (a b)"), op=ALU.mult, reads=[sg, OML], writes=[sg])
                        kb.op('dve', 'tensor_tensor', out=sg[:], in0=sg[:], in1=LB[:].rearrange("p a b -> p (a b)"), op=ALU.add, reads=[sg, LB], writes=[sg])
                    lfo = lfs.get()
                    kb.op('act', 'activation', out=lfo[:], in_=sg[:], func=AF.Ln, reads=[sg], writes=[lfo])
                    kb.dma('pool', self.lf.ap()[tok, :], lfo[:], reads=[lfo])
                    kko = kks.get()
                    kb.op('dve', 'tensor_scalar', out=kko[:], in0=sg[:], scalar1=-1.0, scalar2=1.0, op0=ALU.mult, op1=ALU.add, reads=[sg], writes=[kko])
                    kb.dma('pool', self.kk.ap()[tok, :], kko[:], reads=[kko])
                    p, pv = tr_bf(kko[:], kko, 8)
                    kkTo = kkTs.get()
                    kb.op('act', 'copy', out=kkTo[:], in_=pv[:, 0:8, :], reads=[p], writes=[kkTo])
                    kb.dma('pool', self.kkT.ap()[:, tok].rearrange("(j p) t -> p j t", p=128), kkTo[:], reads=[kkTo])
                    pih = proj_tok(uT, j, 2304, 512)
                    iho = ihs.get()
                    kb.op('dve', 'tensor_copy', out=iho[:], in_=pih[:], reads=[pih], writes=[iho])
                    kb.dma('pool', self.ih.ap()[tok, :], iho[:], reads=[iho])
                    pgh = proj_tok(uT, j, 2816, 512)
                    gho = ghs.get()
                    kb.op('act', 'activation', out=gho[:], in_=pgh[:], func=AF.Silu, reads=[pgh], writes=[gho])
                    kb.dma('pool', self.sgh.ap()[tok, :], gho[:], reads=[gho])
            kb.stage_end()

    def stageB(self, l, dr):
        kb = self.kb
        cp = self.cp
        with ExitStack() as st:
            sb = lambda shape, dt, name: kb.sb(shape, dt, name, stack=st)
            P2 = lambda f, n=2: Pool([f() for _ in range(n)])
            TI = cp[:, (C_TIF if dr == 0 else C_TIB):(C_TIF if dr == 0 else C_TIB) + 128]
            TS = cp[:, (C_TSF if dr == 0 else C_TSB):(C_TSF if dr == 0 else C_TSB) + 128]
            MK = self.mfw if dr == 0 else self.mbw
            S = sb([128, 4, 128], F32, 'S')
            Sb = sb([128, 4, 128], BF16, 'Sb')
            kb.op('dve', 'memset', ap=S[:], constant=0.0, writes=[S])
            kb.op('dve', 'memset', ap=Sb[:], constant=0.0, writes=[Sb])
            lfs = P2(lambda: sb([128, 512], F32, 'lf'))
            kks = P2(lambda: sb([128, 512], BF16, 'kk'))
            kkTs = P2(lambda: sb([128, 4, 128], BF16, 'kkT'))
            qTs = P2(lambda: sb([128, 4, 128], BF16, 'qT'))
            ihs = P2(lambda: sb([128, 512], BF16, 'ih'))
            cumSs = P2(lambda: sb([128, 4, 128], F32, 'cumS'))
            eEs = P2(lambda: sb([128, 512], F32, 'eE'))
            khats = P2(lambda: sb([128, 512], BF16, 'khat'))
            Rqs = P2(lambda: sb([128, 4, 4], F32, 'Rq'))
            for rq in Rqs.bufs:
                kb.op('dve', 'memset', ap=rq[:], constant=0.0, writes=[rq])
            argqs = P2(lambda: sb([128, 4, 128], F32, 'argq'))
            qts = P2(lambda: sb([128, 4, 128], BF16, 'qt'))
            eSs = P2(lambda: sb([128, 4, 128], F32, 'eS'))
            qSs = P2(lambda: sb([128, 4, 128], BF16, 'qS'))
            eqs = P2(lambda: sb([128, 4, 128], F32, 'eq'))
            eGs = P2(lambda: sb([128, 4, 128], F32, 'eG'))
            KGs = P2(lambda: sb([128, 4, 128], BF16, 'KG'))
            Ffs = P2(lambda: sb([128, 4, 4, 4], F32, 'Ff'))
            NEG = sb([128, 4, 4], F32, 'NEG')
            kb.op('dve', 'memset', ap=NEG[:], constant=0.0, writes=[NEG])
            for a_ in range(4):
                for b_ in range(4):
                    if (b_ > a_) if dr == 0 else (b_ < a_):
                        kb.op('dve', 'memset', ap=NEG[:, a_, b_:b_ + 1], constant=-30000.0, writes=[NEG])
            Kts = P2(lambda: sb([128, 4, 4, 128], BF16, 'Kt'))
            scTs = P2(lambda: sb([128, 4, 128], BF16, 'scT'))
            etots = P2(lambda: sb([128, 4], F32, 'etot'))
            osbs = P2(lambda: sb([128, 4, 128], F32, 'osb'))
            pp = Pool([kb.ps([128, 512], F32, 'ppB', stack=st) for _ in range(8)])
            if dr == 1:
                ofws = P2(lambda: sb([128, 512], F32, 'ofw'))
                sghs = P2(lambda: sb([128, 512], BF16, 'sgh'))
                NG = self.bcast_row(st, self.norm_g.ap()[l], 512, 'NG')
                sqs = P2(lambda: sb([128, 128], F32, 'sq'))
                sss = P2(lambda: sb([128, 4], F32, 'ss'))
                hgs = P2(lambda: sb([128, 4, 128], F32, 'hgf'))
                hgbs = P2(lambda: sb([128, 512], BF16, 'hgb'))
                hgTs = P2(lambda: sb([128, 4, 128], BF16, 'hgT'))
            order = list(range(NT)) if dr == 0 else [1, 0] + list(range(NT - 1, 1, -1))
            last = 127 if dr == 0 else 0
            for ti in order:
                tok = slice(ti * 128, (ti + 1) * 128)
                cs = slice(dr * 512, (dr + 1) * 512)
                lf, kk, kkT, qT, ih = lfs.get(), kks.get(), kkTs.get(), qTs.get(), ihs.get()
                kb.dma('sp', lf[:], self.lf.ap()[tok, cs], writes=[lf])
                kb.dma('sp', kk[:], self.kk.ap()[tok, cs], writes=[kk])
                kb.dma('sp', kkT[:], self.kkT.ap()[cs, tok].rearrange("(h k) t -> k h t", k=128), writes=[kkT])
                kb.dma('sp', qT[:], self.qhT.ap()[:, tok].rearrange("(h k) t -> k h t", k=128), writes=[qT])
                kb.dma('sp', ih[:], self.ih.ap()[tok, :], writes=[ih])
                if dr == 1:
                    ofw, sgh = ofws.get(), sghs.get()
                    kb.dma('sp', ofw[:], self.ofw.ap()[tok, :], writes=[ofw])
                    kb.dma('sp', sgh[:], self.sgh.ap()[tok, :], writes=[sgh])
                cps = pp.get()
                cpv = cps[:].rearrange("p (h t) -> p h t", h=4)
                for h in range(4):
                    kb.op('pe', 'matmul', out=cpv[:, h, :], lhsT=lf[:, h * 128:(h + 1) * 128], rhs=TI, start=True, stop=True,
                          reads=[lf, cp], writes=[cps], inc=(h == 3))
                eps_ = pp.get()
                kb.op('pe', 'matmul', out=eps_[:], lhsT=TS, rhs=lf[:], start=True, stop=True, reads=[lf, cp], writes=[eps_])
                cumS = cumSs.get()
                kb.op('act', 'copy', out=cumS[:], in_=cpv, reads=[cps], writes=[cumS])
                eE = eEs.get()
                kb.op('act', 'activation', out=eE[:], in_=eps_[:], func=AF.Exp, reads=[eps_], writes=[eE])
                khat = khats.get()
                kb.op('dve', 'tensor_tensor', out=khat[:], in0=kk[:], in1=eE[:], op=ALU.mult, reads=[kk, eE], writes=[khat])
                Rq = Rqs.get()
                c4 = cumS[:].rearrange("p h (a c) -> p h a c", a=4)
                if dr == 0:
                    kb.op('pool', 'tensor_copy', out=Rq[:, :, 1:4], in_=c4[:, :, 0:3, 31], reads=[cumS], writes=[Rq])
                else:
                    kb.op('pool', 'tensor_copy', out=Rq[:, :, 0:3], in_=c4[:, :, 1:4, 0], reads=[cumS], writes=[Rq])
                argq = argqs.get()
                kb.op('dve', 'tensor_tensor', out=argq[:].rearrange("p h (a c) -> p h a c", a=4), in0=c4,
                      in1=Rq[:].unsqueeze(3).to_broadcast([128, 4, 4, 32]), op=ALU.subtract, reads=[cumS, Rq], writes=[argq])
                kb.op('dve', 'tensor_scalar_max', out=argq[:], in0=argq[:], scalar1=-69.0, reads=[argq], writes=[argq])
                eq = eqs.get()
                kb.op('act', 'activation', out=eq[:], in_=argq[:], func=AF.Exp, reads=[argq], writes=[eq])
                qt = qts.get()
                kb.op('dve', 'tensor_tensor', out=qt[:], in0=eq[:], in1=qT[:], op=ALU.mult, reads=[eq, qT], writes=[qt])
                eG = eGs.get()
                kb.op('act', 'activation', out=eG[:], in_=argq[:], func=AF.Exp, scale=-1.0, reads=[argq], writes=[eG])
                KG = KGs.get()
                kb.op('dve', 'tensor_tensor', out=KG[:], in0=eG[:], in1=kkT[:], op=ALU.mult, reads=[eG, kkT], writes=[KG])
                eS = eSs.get()
                kb.op('act', 'activation', out=eS[:], in_=cumS[:], func=AF.Exp, reads=[cumS], writes=[eS])
                qS = qSs.get()
                kb.op('dve', 'tensor_tensor', out=qS[:], in0=eS[:], in1=qT[:], op=ALU.mult, reads=[eS, qT], writes=[qS])
                Ff = Ffs.get()
                kb.op('pool', 'tensor_tensor', out=Ff[:], in0=Rq[:].unsqueeze(3).to_broadcast([128, 4, 4, 4]),
                      in1=Rq[:].unsqueeze(2).to_broadcast([128, 4, 4, 4]), op=ALU.subtract, reads=[Rq], writes=[Ff])
                kb.op('pool', 'tensor_tensor', out=Ff[:], in0=Ff[:], in1=NEG[:].unsqueeze(1).to_broadcast([128, 4, 4, 4]), op=ALU.add,
                      reads=[Ff, NEG], writes=[Ff])
                kb.op('act', 'activation', out=Ff[:], in_=Ff[:], func=AF.Exp, reads=[Ff], writes=[Ff])
                Kt = Kts.get()
                for h in range(4):
                    kb.op('dve', 'tensor_tensor', out=Kt[:, h].rearrange("p a (b c) -> p a b c", b=4),
                          in0=KG[:, h, :].rearrange("p (b c) -> p b c", b=4).unsqueeze(1).to_broadcast([128, 4, 4, 32]),
                          in1=Ff[:, h].unsqueeze(3).to_broadcast([128, 4, 4, 32]), op=ALU.mult, reads=[KG, Ff], writes=[Kt])
                sps = pp.get()
                spv = sps[:].rearrange("p (h t) -> p h t", h=4)
                for h in range(4):
                    for a in range(4):
                        kb.op('pe', 'matmul', out=spv[:, h, a * 32:(a + 1) * 32], lhsT=Kt[:, h, a, :], rhs=qt[:, h, a * 32:(a + 1) * 32],
                              start=True, stop=True, reads=[Kt, qt], writes=[sps], inc=(h == 3 and a == 3))
                scT = scTs.get()
                kb.op('dve', 'tensor_tensor', out=scT[:], in0=spv, in1=MK[:].unsqueeze(1).to_broadcast([128, 4, 128]), op=ALU.mult,
                      reads=[sps, MK], writes=[scT])
                ops_ = pp.get()
                opv = ops_[:].rearrange("p (h t) -> p h t", h=4)
                for h in range(4):
                    kb.op('pe', 'matmul', out=opv[:, h, :], lhsT=qS[:, h, :], rhs=Sb[:, h, :], start=True, stop=False,
                          reads=[qS, Sb], writes=[ops_], inc=False)
                    kb.op('pe', 'matmul', out=opv[:, h, :], lhsT=scT[:, h, :], rhs=ih[:, h * 128:(h + 1) * 128], start=False, stop=True,
                          reads=[scT, ih], writes=[ops_], inc=(h == 3))
                nps = pp.get()
                npv = nps[:].rearrange("p (h t) -> p h t", h=4)
                for h in range(4):
                    kb.op('pe', 'matmul', out=npv[:, h, :], lhsT=khat[:, h * 128:(h + 1) * 128], rhs=ih[:, h * 128:(h + 1) * 128], start=True, stop=True,
                          reads=[khat, ih], writes=[nps], inc=(h == 3))
                etot = etots.get()
                kb.op('act', 'activation', out=etot[:], in_=cumS[:, :, last], func=AF.Exp, reads=[cumS], writes=[etot])
                for h in range(4):
                    kb.op('dve', 'scalar_tensor_tensor', out=S[:, h, :], in0=S[:, h, :], scalar=etot[:, h:h + 1], in1=npv[:, h, :],
                          op0=ALU.mult, op1=ALU.add, reads=[S, etot, nps], writes=[S])
                kb.op('act', 'copy', out=Sb[:], in_=S[:], reads=[S], writes=[Sb])
                if dr == 0:
                    osb = osbs.get()
                    kb.op('act', 'copy', out=osb[:], in_=opv, reads=[ops_], writes=[osb])
                    kb.dma('pool', self.ofw.ap()[tok, :], osb[:].rearrange("p h t -> p (h t)"), reads=[osb])
                else:
                    osb = osbs.get()
                    kb.op('dve', 'tensor_tensor', out=osb[:], in0=opv, in1=ofw[:].rearrange("p (h t) -> p h t", h=4), op=ALU.add,
                          reads=[ops_, ofw], writes=[osb])
                    ss = sss.get()
                    sq = sqs.get()
                    for h in range(4):
                        kb.op('act', 'activation', out=sq[:], in_=osb[:, h, :], func=AF.Square, accum_out=ss[:, h:h + 1],
                              reads=[osb], writes=[sq, ss])
                    kb.op('act', 'activation', out=ss[:], in_=ss[:], func=AF.Sqrt, bias=LN_EPS, scale=1.0 / 128, reads=[ss], writes=[ss])
                    kb.op('dve', 'reciprocal', out=ss[:], in_=ss[:], reads=[ss], writes=[ss])
                    hg = hgs.get()
                    kb.op('dve', 'tensor_tensor', out=hg[:], in0=osb[:], in1=ss[:].unsqueeze(2).to_broadcast([128, 4, 128]), op=ALU.mult,
                          reads=[osb, ss], writes=[hg])
                    hgf = hg[:].rearrange("p h t -> p (h t)")
                    kb.op('dve', 'tensor_tensor', out=hgf, in0=hgf, in1=NG[:], op=ALU.mult, reads=[hg, NG], writes=[hg])
                    hgb = hgbs.get()
                    kb.op('dve', 'tensor_tensor', out=hgb[:], in0=hgf, in1=sgh[:], op=ALU.mult, reads=[hg, sgh], writes=[hgb])
                    tps = pp.get()
                    tpv = tps[:].bitcast(BF16).rearrange("p (a b) -> p a b", b=128)
                    for h in range(4):
                        kb.op('pe', 'transpose', out=tpv[:, h, :], in_=hgb[:, h * 128:(h + 1) * 128], identity=self.idb[:],
                              reads=[hgb, self.idb], writes=[tps], inc=(h == 3))
                    hgT = hgTs.get()
                    kb.op('act', 'copy', out=hgT[:], in_=tpv[:, 0:4, :], reads=[tps], writes=[hgT])
                    kb.dma('pool', self.hgT.ap()[:, tok].rearrange("(h k) t -> k h t", k=128), hgT[:], reads=[hgT])
            kb.stage_end()

    def stageC(self, l):
        kb = self.kb
        with ExitStack() as st:
            sb = lambda shape, dt, name: kb.sb(shape, dt, name, stack=st)
            P2 = lambda f, n=2: Pool([f() for _ in range(n)])
            kTd = sb([128, 2, NTOK], BF16, 'kTd')
            for half in range(2):
                kb.dma('sp', kTd[half * 64:(half + 1) * 64, :, :], self.kT.ap().rearrange("(g d) t -> d g t", d=64), writes=[kTd])
            vaug = sb([128, NT, 2, 65], BF16, 'vaug')
            kb.op('dve', 'memset', ap=vaug[:, :, :, 64:65], constant=1.0, writes=[vaug])
            for i0 in range(0, NT, 11):
                for g in range(2):
                    kb.dma('sp', vaug[:, i0:i0 + 11, g, 0:64],
                           self.va.ap()[i0 * 128:(i0 + 11) * 128, g * 64:(g + 1) * 64].rearrange("(i s) d -> s i d", s=128), writes=[vaug])
            snk = sb([128, 8], F32, 'snk')
            kb.dma('sp', snk[:], self.attn_sink.ap()[l].partition_broadcast(128), writes=[snk])
            kb.op('act', 'activation', out=snk[:], in_=snk[:], func=AF.Exp, reads=[snk], writes=[snk])
            qTs = P2(lambda: sb([128, 4, 128], BF16, 'qTc'))
            es = P2(lambda: sb([128, 5, 128], BF16, 'esb'), 3)
            dens = P2(lambda: sb([128, 8], F32, 'den'))
            ats = P2(lambda: sb([128, 8, 64], BF16, 'att'))
            aTs = P2(lambda: sb([128, 4, 128], BF16, 'aT'))
            stp = Pool([kb.ps([128, 2, 512], F32, 'stp', stack=st) for _ in range(2)])
            ops = Pool([kb.ps([128, 2, 512], F32, 'opc', stack=st) for _ in range(1)])
            tpp = Pool([kb.ps([128, 512], F32, 'tpc', stack=st) for _ in range(2)])
            for ti in range(NT):
                tok = slice(ti * 128, (ti + 1) * 128)
                qT = qTs.get()
                kb.dma('sp', qT[:], self.qT.ap()[:, tok].rearrange("(j p) t -> p j t", p=128), writes=[qT])
                if ti < 2:
                    blocks = [(0, None), (1, None)]
                else:
                    blocks = [(0, None), (1, None)]
                    if ti - 1 >= 2:
                        blocks.append((ti - 1, self.ngp))
                    blocks.append((ti, None))
                    if ti + 1 < NT:
                        blocks.append((ti + 1, self.ngn))
                nb = len(blocks)
                op_ = ops.get()
                for h in range(8):
                    g, j, half = h // 4, h // 2, h % 2
                    ps = slice(half * 64, (half + 1) * 64)
                    sp_ = stp.get()
                    spv = sp_[:].rearrange("p a b -> p (a b)").rearrange("p (k t) -> p k t", t=128)
                    for bi, (kt, mk) in enumerate(blocks):
                        kb.op('pe', 'matmul', out=spv[:, bi, :], lhsT=kTd[ps, g, kt * 128:(kt + 1) * 128], rhs=qT[ps, j, :],
                              start=True, stop=(mk is None), reads=[kTd, qT], writes=[sp_], inc=(bi == nb - 1 and mk is None))
                        if mk is not None:
                            kb.op('pe', 'matmul', out=spv[:, bi, :], lhsT=self.idb[:], rhs=mk[:], start=False, stop=True,
                                  reads=[self.idb, mk], writes=[sp_], inc=(bi == nb - 1))
                    e = es.get()
                    kb.op('act', 'activation', out=e[:, 0:nb, :], in_=spv[:, 0:nb, :], func=AF.Exp, scale=0.125, reads=[sp_], writes=[e])
                    oc = (h % 4) * 65
                    for bi, (kt, _) in enumerate(blocks):
                        kb.op('pe', 'matmul', out=op_[:, h // 4, oc:oc + 65], lhsT=e[:, bi, :], rhs=vaug[:, kt, g, :],
                              start=(bi == 0), stop=(bi == nb - 1), reads=[e, vaug], writes=[op_], inc=(bi == nb - 1))
                ov = op_[:, :, 0:260].rearrange("p b (h d) -> p b h d", d=65)
                den = dens.get()
                kb.op('dve', 'tensor_tensor', out=den[:].rearrange("p (b h) -> p b h", b=2), in0=ov[:, :, :, 64],
                      in1=snk[:].rearrange("p (b h) -> p b h", b=2), op=ALU.add, reads=[op_, snk], writes=[den])
                kb.op('dve', 'reciprocal', out=den[:], in_=den[:], reads=[den], writes=[den])
                at = ats.get()
                for b in range(2):
                    kb.op('dve', 'tensor_tensor', out=at[:, b * 4:(b + 1) * 4, :], in0=ov[:, b, :, 0:64],
                          in1=den[:, b * 4:(b + 1) * 4].unsqueeze(2).to_broadcast([128, 4, 64]), op=ALU.mult, reads=[op_, den], writes=[at])
                tp = tpp.get()
                tpv = tp[:].bitcast(BF16).rearrange("p (a b) -> p a b", b=128)
                af = at[:].rearrange("p h d -> p (h d)")
                for j in range(4):
                    kb.op('pe', 'transpose', out=tpv[:, j, :], in_=af[:, j * 128:(j + 1) * 128], identity=self.idb[:],
                          reads=[at, self.idb], writes=[tp], inc=(j == 3))
                aT = aTs.get()
                kb.op('act', 'copy', out=aT[:], in_=tpv[:, 0:4, :], reads=[tp], writes=[aT])
                kb.dma('pool', self.attnT.ap()[:, tok].rearrange("(j p) t -> p j t", p=128), aT[:], reads=[aT])
            kb.stage_end()

    def load_w_bf16(self, st, dram_ap, kc, n, name):
        kb = self.kb
        w = kb.sb([128, kc, n], BF16, name, stack=st)
        if not hasattr(self, '_wst') or self._wst_stage is not st:
            self._wst = Pool([kb.sb([128, 2048], F32, 'wstg', stack=st) for _ in range(2)])
            self._wst_stage = st
            self._wi = 0
        src = dram_ap.rearrange("(kc k) n -> k kc n", k=128)
        step = max(1, 2048 // n)
        for k0 in range(0, kc, step):
            k1 = min(kc, k0 + step)
            ws = self._wst.get()
            wv = ws[:, 0:(k1 - k0) * n].rearrange("p (a b) -> p a b", b=n)
            kb.dma('sp', wv, src[:, k0:k1, :], writes=[ws])
            self._wi += 1
            if self._wi % 2:
                kb.op('dve', 'tensor_copy', out=w[:, k0:k1, :], in_=wv, reads=[ws], writes=[w])
            else:
                kb.op('act', 'copy', out=w[:, k0:k1, :], in_=wv, reads=[ws], writes=[w])
        return w

    def stageD(self, l, xin):
        kb = self.kb
        cp = self.cp
        with ExitStack() as st:
            sb = lambda shape, dt, name: kb.sb(shape, dt, name, stack=st)
            P2 = lambda f, n=2: Pool([f() for _ in range(n)])
            wba = self.load_w_bf16(st, self.w_ba.ap()[l], 4, D, 'wba')
            wbh = self.load_w_bf16(st, self.w_bh.ap()[l], 4, D, 'wbh')
            wo = self.load_w_bf16(st, self.w_out.ap()[l], 8, D, 'wo')
            wr = sb([128, 8, NEXP], F32, 'wr')
            kb.dma('sp', wr[:], self.w_router.ap()[l].rearrange("(kc k) n -> k kc n", k=128), writes=[wr])
            G1 = [self.modtile(st, r, 2, 'G1') for r in range(2)]
            A2 = [self.modtile(st, r, 4, 'A2') for r in range(2)]
            B2 = [self.modtile(st, r, 3, 'B2') for r in range(2)]
            LG = self.bcast_row(st, self.ln_g.ap()[l, 0], D, 'LG')
            LBt = self.bcast_row(st, self.ln_b.ap()[l, 0], D, 'LBt')
            aTs = P2(lambda: sb([128, 4, 512], BF16, 'aTg'), 1)
            hTs = P2(lambda: sb([128, 4, 512], BF16, 'hTg'), 1)
            gas = P2(lambda: sb([128, 8, 512], BF16, 'gag'), 1)
            gbs = P2(lambda: sb([128, 8, 512], BF16, 'gbg'), 1)
            mTs = P2(lambda: sb([128, 8, 512], BF16, 'mT'), 1)
            t1s = P2(lambda: sb([128, 512], F32, 't1'))
            t2s = P2(lambda: sb([128, 512], F32, 't2'))
            xts = P2(lambda: sb([128, D], F32, 'xtd'))
            rrs = P2(lambda: sb([128, D], F32, 'rr'))
            x1s = P2(lambda: sb([128, D], F32, 'x1o'))
            u2s = P2(lambda: sb([128, D], F32, 'u2f'))
            u2bs = P2(lambda: sb([128, D], BF16, 'u2b'))
            u2Ts = P2(lambda: sb([128, 8, 128], F32, 'u2T'))
            stt = P2(lambda: sb([128, 2, 6], F32, 'sttd'), 4)
            mvs = P2(lambda: sb([128, 2], F32, 'mvd'), 4)
            rss = P2(lambda: sb([128, 1], F32, 'rsd'), 4)
            nms = P2(lambda: sb([128, 1], F32, 'nmd'), 4)
            mxs = P2(lambda: sb([128, 1], F32, 'mx'))
            sms = P2(lambda: sb([128, 1], F32, 'sm'))
            exs = P2(lambda: sb([128, NEXP], F32, 'ex'))
            afs = P2(lambda: sb([NEXP, 128], F32, 'af'))
            pa = Pool([kb.ps([128, 512], F32, 'pa', stack=st) for _ in range(2)])
            py = Pool([kb.ps([128, 2, 512], F32, 'py', stack=st) for _ in range(1)])
            pt = Pool([kb.ps([128, 2, 512], F32, 'pt', stack=st) for _ in range(1)])
            pl = Pool([kb.ps([128, 512], F32, 'pl', stack=st) for _ in range(2)])
            for (t0, nt) in groups():
                N = nt * 128
                r = 1 if t0 < 2 else 0
                gtok = slice(t0 * 128, t0 * 128 + N)
                aT, hT, ga, gb, mT = aTs.get(), hTs.get(), gas.get(), gbs.get(), mTs.get()
                kb.dma('sp', aT[:, :, 0:N], self.attnT.ap()[:, gtok].rearrange("(j p) t -> p j t", p=128), writes=[aT])
                kb.dma('sp', hT[:, :, 0:N], self.hgT.ap()[:, gtok].rearrange("(j p) t -> p j t", p=128), writes=[hT])
                kb.dma('sp', ga[:, :, 0:N], self.sgaT.ap()[:, gtok].rearrange("(j p) t -> p j t", p=128), writes=[ga])
                kb.dma('sp', gb[:, :, 0:N], self.sgbT.ap()[:, gtok].rearrange("(j p) t -> p j t", p=128), writes=[gb])
                for n in range(8):
                    pA, pH = pa.get(), pa.get()
                    for kc in range(4):
                        kb.op('pe', 'matmul', out=pA[:, 0:N], lhsT=wba[:, kc, n * 128:(n + 1) * 128], rhs=aT[:, kc, 0:N],
                              start=(kc == 0), stop=(kc == 3), reads=[wba, aT], writes=[pA], inc=(kc == 3))
                    for kc in range(4):
                        kb.op('pe', 'matmul', out=pH[:, 0:N], lhsT=wbh[:, kc, n * 128:(n + 1) * 128], rhs=hT[:, kc, 0:N],
                              start=(kc == 0), stop=(kc == 3), reads=[wbh, hT], writes=[pH], inc=(kc == 3))
                    t1, t2 = t1s.get(), t2s.get()
                    kb.op('dve', 'tensor_tensor', out=t1[:, 0:N], in0=pA[:, 0:N], in1=ga[:, n, 0:N], op=ALU.mult, reads=[pA, ga], writes=[t1])
                    kb.op('dve', 'tensor_tensor', out=t2[:, 0:N], in0=pH[:, 0:N], in1=gb[:, n, 0:N], op=ALU.mult, reads=[pH, gb], writes=[t2])
                    kb.op('dve', 'tensor_tensor', out=mT[:, n, 0:N], in0=t1[:, 0:N], in1=t2[:, 0:N], op=ALU.add, reads=[t1, t2], writes=[mT])
                u2_of = {}
                def part1(j):
                        ti = t0 + j
                        tok = slice(ti * 128, (ti + 1) * 128)
                        xt = xts.get()
                        kb.dma('sp', xt[:], xin.ap()[tok, :], writes=[xt])
                        yp = py.get()
                        for hf in range(2):
                            for kc in range(8):
                                kb.op('pe', 'matmul', out=yp[:, hf, :], lhsT=mT[:, kc, j * 128:(j + 1) * 128], rhs=wo[:, kc, hf * 512:(hf + 1) * 512],
                                      start=(kc == 0), stop=(kc == 7), reads=[mT, wo], writes=[yp], inc=(kc == 7))
                        rr = rrs.get()
                        kb.op('dve', 'tensor_tensor', out=rr[:], in0=yp[:].rearrange("p a b -> p (a b)"), in1=G1[r][:], op=ALU.mult, reads=[yp, G1[r]], writes=[rr])
                        kb.op('dve', 'scalar_tensor_tensor', out=rr[:], in0=xt[:], scalar=ALPHA, in1=rr[:], op0=ALU.mult, op1=ALU.add, reads=[xt, rr], writes=[rr])
                        s6, mv, rs, nm = stt.get(), mvs.get(), rss.get(), nms.get()
                        self.ln_stats(st, rr, rs, nm, s6, mv)
                        x1 = x1s.get()
                        kb.op('act', 'activation', out=x1[:], in_=rr[:], func=AF.Identity, bias=nm[:], scale=rs[:], reads=[rr, nm, rs], writes=[x1])
                        kb.op('dve', 'tensor_tensor', out=x1[:], in0=x1[:], in1=LG[:], op=ALU.mult, reads=[x1, LG], writes=[x1])
                        kb.op('dve', 'tensor_tensor', out=x1[:], in0=x1[:], in1=LBt[:], op=ALU.add, reads=[x1, LBt], writes=[x1])
                        kb.dma('pool', self.x1.ap()[tok, :], x1[:], reads=[x1])
                        s6, mv, rs, nm = stt.get(), mvs.get(), rss.get(), nms.get()
                        self.ln_stats(st, x1, rs, nm, s6, mv)
                        u2 = u2s.get()
                        kb.op('act', 'activation', out=u2[:], in_=x1[:], func=AF.Identity, bias=nm[:], scale=rs[:], reads=[x1, nm, rs], writes=[u2])
                        kb.op('dve', 'tensor_tensor', out=u2[:], in0=u2[:], in1=A2[r][:], op=ALU.mult, reads=[u2, A2[r]], writes=[u2])
                        kb.op('dve', 'tensor_tensor', out=u2[:], in0=u2[:], in1=B2[r][:], op=ALU.add, reads=[u2, B2[r]], writes=[u2])
                        u2b = u2bs.get()
                        kb.op('act', 'copy', out=u2b[:], in_=u2[:], reads=[u2], writes=[u2b])
                        kb.dma('pool', self.u2.ap()[tok, :], u2b[:], reads=[u2b])
                        u2_of[j] = u2
                def part2(j):
                        ti = t0 + j
                        tok = slice(ti * 128, (ti + 1) * 128)
                        u2 = u2_of[j]
                        tp = pt.get()
                        tpv = tp[:].rearrange("p a b -> p (a b)").rearrange("p (k t) -> p k t", t=128)
                        for kc in range(8):
                            kb.op('pe', 'transpose', out=tpv[:, kc, :], in_=u2[:, kc * 128:(kc + 1) * 128], identity=cp[:, C_ID:C_ID + 128],
                                  reads=[u2, cp], writes=[tp], inc=(kc == 7))
                        u2T = u2Ts.get()
                        kb.op('act', 'copy', out=u2T[:], in_=tpv, reads=[tp], writes=[u2T])
                        lp = pl.get()
                        for kc in range(8):
                            kb.op('pe', 'matmul', out=lp[:, 0:NEXP], lhsT=u2T[:, kc, :], rhs=wr[:, kc, :], start=(kc == 0), stop=(kc == 7),
                                  reads=[u2T, wr], writes=[lp], inc=(kc == 7))
                        mx, sm, ex = mxs.get(), sms.get(), exs.get()
                        kb.op('dve', 'tensor_reduce', out=mx[:], in_=lp[:, 0:NEXP], axis=AX.X, op=ALU.max, negate=True, reads=[lp], writes=[mx])
                        kb.op('act', 'activation', out=ex[:], in_=lp[:, 0:NEXP], func=AF.Exp, bias=mx[:], scale=1.0, accum_out=sm[:],
                              reads=[lp, mx], writes=[ex, sm])
                        kb.op('dve', 'reciprocal', out=sm[:], in_=sm[:], reads=[sm], writes=[sm])
                        kb.op('dve', 'tensor_scalar', out=ex[:], in0=ex[:], scalar1=sm[:], scalar2=None, op0=ALU.mult, reads=[ex, sm], writes=[ex])
                        ap_ = pl.get()
                        kb.op('pe', 'transpose', out=ap_[0:NEXP, 0:128], in_=ex[:], identity=cp[:, C_ID:C_ID + 128], reads=[ex, cp], writes=[ap_])
                        af = afs.get()
                        kb.op('act', 'copy', out=af[:], in_=ap_[0:NEXP, 0:128], reads=[ap_], writes=[af])
                        kb.dma('pool', self.affT.ap()[:, tok], af[:], reads=[af])
                part1(0)
                for j in range(nt):
                    if j + 1 < nt:
                        part1(j + 1)
                    part2(j)
            kb.stage_end()

    NROUND = 28

    def stageE(self, l, first):
        kb = self.kb
        cp = self.cp
        NR = self.NROUND
        NC_ = NR * 8
        with ExitStack() as st:
            sb = lambda shape, dt, name: kb.sb(shape, dt, name, stack=st)
            if first:
                zt = sb([128, 2 * (NEXP * LROW + 256) // 128], F32, 'zt')
                kb.op('pool', 'memset', ap=zt[:], constant=0.0, writes=[zt])
                zf = kb.dma('sp', self.pairs.ap().rearrange("(p a) c -> p (a c)", p=128), zt[:], reads=[zt])
            affL = sb([128, 1024], F32, 'affL')
            for e in range(NEXP):
                kb.dma('sp', affL[e * 8:(e + 1) * 8, :], self.affT.ap()[e, LCTX:].rearrange("(s n) -> s n", s=8), writes=[affL])
            affC = sb([NEXP, LCTX], F32, 'affC')
            kb.dma('sp', affC[:], self.affT.ap()[:, 0:LCTX], writes=[affC])
            lo = sb([128, 1], F32, 'lo')
            mid = sb([128, 1], F32, 'mid')
            cnt = sb([128, 1], F32, 'cnt')
            ge = sb([128, 1], F32, 'ge')
            junk = sb([128, 1024], F32, 'junk')
            pp = Pool([kb.ps([128, 512], F32, 'ppE', stack=st) for _ in range(2)])
            kb.op('dve', 'memset', ap=lo[:], constant=0.0, writes=[lo])
            for it in range(28):
                c = 2.0 ** -(it + 1)
                kb.op('dve', 'tensor_scalar_add', out=mid[:], in0=lo[:], scalar1=c, reads=[lo], writes=[mid])
                kb.op('dve', 'tensor_scalar', out=junk[:], in0=affL[:], scalar1=mid[:], scalar2=None, op0=ALU.is_ge, op1=ALU.add,
                      accum_out=cnt[:], reads=[affL, mid], writes=[junk, cnt])
                tp = pp.get()
                kb.op('pe', 'matmul', out=tp[:, 0:1], lhsT=cp[:, C_BD:C_BD + 128], rhs=cnt[:], start=True, stop=True, reads=[cp, cnt], writes=[tp])
                kb.op('dve', 'tensor_scalar', out=ge[:], in0=tp[:, 0:1], scalar1=CAP - 0.5, scalar2=c, op0=ALU.is_ge, op1=ALU.mult,
                      reads=[tp], writes=[ge])
                kb.op('dve', 'tensor_tensor', out=lo[:], in0=lo[:], in1=ge[:], op=ALU.add, reads=[lo, ge], writes=[lo])
            kb.op('dve', 'tensor_scalar', out=junk[:], in0=affL[:], scalar1=lo[:], scalar2=None, op0=ALU.is_ge, op1=ALU.add,
                  accum_out=cnt[:], reads=[affL, lo], writes=[junk, cnt])
            tp = pp.get()
            kb.op('pe', 'matmul', out=tp[:, 0:1], lhsT=cp[:, C_OFF:C_OFF + 128], rhs=cnt[:], start=True, stop=True, reads=[cp, cnt], writes=[tp])
            off = sb([128, 1], F32, 'off')
            kb.op('dve', 'tensor_copy', out=off[:], in_=tp[:, 0:1], reads=[tp], writes=[off])
            work = junk
            kb.op('dve', 'tensor_copy', out=work[:], in_=affL[:], reads=[affL], writes=[work])
            cv = sb([128, NC_], F32, 'cv')
            ci = sb([128, NC_], U32, 'ci')
            for r in range(NR):
                rs_ = slice(r * 8, (r + 1) * 8)
                kb.op('dve', 'max', out=cv[:, rs_], in_=work[:], reads=[work], writes=[cv])
                kb.op('dve', 'max_index', out=ci[:, rs_], in_max=cv[:, rs_], in_values=work[:], reads=[cv, work], writes=[ci])
                kb.op('dve', 'match_replace', out=work[:], in_to_replace=cv[:, rs_], in_values=work[:], imm_value=-1.0, reads=[cv, work], writes=[work])
            rI = cp[:, C_IOTA:C_IOTA + NC_]
            v1 = sb([128, NC_], F32, 'v1')
            sl = sb([128, NC_], F32, 'sl')
            v2 = sb([128, NC_], F32, 'v2')
            kb.op('dve', 'tensor_scalar', out=v1[:], in0=rI, scalar1=cnt[:], scalar2=None, op0=ALU.is_lt, reads=[cp, cnt], writes=[v1])
            kb.op('dve', 'tensor_scalar', out=sl[:], in0=rI, scalar1=off[:], scalar2=None, op0=ALU.add, reads=[cp, off], writes=[sl])
            kb.op('dve', 'tensor_scalar', out=v2[:], in0=sl[:], scalar1=CAP - 0.5, scalar2=None, op0=ALU.is_lt, reads=[sl], writes=[v2])
            kb.op('dve', 'tensor_tensor', out=v1[:], in0=v1[:], in1=v2[:], op=ALU.mult, reads=[v1, v2], writes=[v1])
            BIG = 1.0e6
            kb.op('dve', 'tensor_scalar', out=sl[:], in0=sl[:], scalar1=cp[:, C_EB:C_EB + 1], scalar2=-BIG, op0=ALU.add, op1=ALU.add, reads=[sl, cp], writes=[sl])
            kb.op('dve', 'tensor_tensor', out=sl[:], in0=sl[:], in1=v1[:], op=ALU.mult, reads=[sl, v1], writes=[sl])
            kb.op('dve', 'tensor_scalar_add', out=sl[:], in0=sl[:], scalar1=BIG, reads=[sl], writes=[sl])
            dst = sb([128, NC_], U32, 'dst')
            kb.op('dve', 'tensor_copy', out=dst[:], in_=sl[:], reads=[sl], writes=[dst])
            PR = sb([128, NC_, 2], F32, 'PR')
            kb.op('dve', 'tensor_copy', out=PR[:, :, 0], in_=ci[:], reads=[ci], writes=[PR])
            kb.op('dve', 'tensor_scalar', out=PR[:, :, 0], in0=PR[:, :, 0], scalar1=cp[:, C_SEGB:C_SEGB + 1], scalar2=None, op0=ALU.add, reads=[PR, cp], writes=[PR])
            kb.op('dve', 'tensor_copy', out=PR[:, :, 1], in_=cv[:], reads=[cv], writes=[PR])
            ptab = self.pairs.ap()
            if first:
                kb.wait_dma('pool', *zf)
                kb.wait_dma('sp', *zf)
            kb._deps('pool', [PR, dst], [])
            for r in range(NC_):
                def fn(e, r=r):
                    return e.indirect_dma_start(out=ptab, out_offset=bass.IndirectOffsetOnAxis(ap=dst[:, r:r + 1], axis=0),
                                                in_=PR[:, r, :], in_offset=None, bounds_check=self.breg(e, NEXP * LROW - 1), oob_is_err=False)
                kb.dma('pool', None, None, reads=[PR], fn=fn)
            workc = sb([NEXP, LCTX], F32, 'workc')
            kb.op('dve', 'tensor_copy', out=workc[:], in_=affC[:], reads=[affC], writes=[workc])
            cvc = sb([NEXP, CAPC], F32, 'cvc')
            cic = sb([NEXP, CAPC], U32, 'cic')
            for r in range(CAPC // 8):
                rs_ = slice(r * 8, (r + 1) * 8)
                kb.op('dve', 'max', out=cvc[:, rs_], in_=workc[:], reads=[workc], writes=[cvc])
                kb.op('dve', 'max_index', out=cic[:, rs_], in_max=cvc[:, rs_], in_values=workc[:], reads=[cvc, workc], writes=[cic])
                kb.op('dve', 'match_replace', out=workc[:], in_to_replace=cvc[:, rs_], in_values=workc[:], imm_value=-1.0, reads=[cvc, workc], writes=[workc])
            PRc = sb([NEXP, CAPC, 2], F32, 'PRc')
            kb.op('dve', 'tensor_copy', out=PRc[:, :, 0], in_=cic[:], reads=[cic], writes=[PRc])
            kb.op('dve', 'tensor_copy', out=PRc[:, :, 1], in_=cvc[:], reads=[cvc], writes=[PRc])
            kb.dma('sp', self.pairs.ap()[0:NEXP * LROW, :].rearrange("(e s) c -> e s c", s=LROW)[:, CAP:LROW, :], PRc[:], reads=[PRc])
            kb.stage_end()

    def stageF(self, l):
        kb = self.kb
        with ExitStack() as st:
            sb = lambda shape, dt, name: kb.sb(shape, dt, name, stack=st)
            P2 = lambda f, n=2: Pool([f() for _ in range(n)])
            zt = sb([128, 2048], F32, 'ztf')
            kb.op('pool', 'memset', ap=zt[:], constant=0.0, writes=[zt])
            accv = self.acc.ap().rearrange("(p a) c -> p (a c)", p=128)
            zsem = None
            for i in range(0, NT * 1024, 2048):
                w = min(2048, NT * 1024 - i)
                zsem = kb.dma('sp', accv[:, i:i + w], zt[:, 0:w], reads=[zt])
            L = sb([128, NEXP, 8, 2], F32, 'L')
            ptab = self.pairs.ap()[0:NEXP * LROW, :].rearrange("(e s) c -> e s c", s=LROW)
            for e in range(NEXP):
                kb.dma('sp', L[:, e], ptab[e, 0:CAP, :].rearrange("(p c) two -> p c two", c=8), writes=[L])
            Lc = sb([CAPC, NEXP, 2], F32, 'Lc')
            kb.dma('sp', Lc[:], ptab[:, CAP:LROW, :].rearrange("e j two -> j e two"), writes=[Lc])
            idx = sb([128, NEXP, 8], I32, 'idx')
            idxc = sb([CAPC, NEXP], I32, 'idxc')
            kb.op('dve', 'tensor_copy', out=idx[:], in_=L[:, :, :, 0], reads=[L], writes=[idx])
            kb.op('dve', 'tensor_copy', out=idxc[:], in_=Lc[:, :, 0], reads=[Lc], writes=[idxc])
            wgs = P2(lambda: sb([128, 8, D], BF16, 'wg'))
            wus = P2(lambda: sb([128, 8, D], BF16, 'wu'))
            wds = P2(lambda: sb([128, 8, D], BF16, 'wd'))
            wst = Pool([sb([128, 2048], F32, 'wstf') for _ in range(2)])
            xss = P2(lambda: sb([128, D], BF16, 'xs'), 3)
            xsTs = P2(lambda: sb([128, 8, 512], BF16, 'xsT'))
            hTs = P2(lambda: sb([128, 8, 512], BF16, 'hTe'), 1)
            sgs = P2(lambda: sb([128, 512], BF16, 'sge'))
            ysbs = P2(lambda: sb([128, D], F32, 'ysb'), 3)
            pg = Pool([kb.ps([128, 512], F32, 'pg', stack=st) for _ in range(4)])
            py = Pool([kb.ps([128, 2, 512], F32, 'pye', stack=st) for _ in range(2)])
            u2d = self.u2.ap()
            accd = self.acc.ap()
            wi = 0
            prev_scatter = [zsem] if zsem else []
            for e in range(NEXP):
                ws3 = []
                for (src, pool_) in ((self.w_gate, wgs), (self.w_up, wus), (self.w_down, wds)):
                    w = pool_.get()
                    sv = src.ap()[l, e].rearrange("(kc k) n -> k kc n", k=128)
                    for k0 in range(0, 8, 2):
                        ws = wst.get()
                        wv = ws[:].rearrange("p (a b) -> p a b", b=D)
                        kb.dma('sp', wv, sv[:, k0:k0 + 2, :], writes=[ws])
                        wi += 1
                        if wi % 2 == 0:
                            kb.op('act', 'copy', out=w[:, k0:k0 + 2, :], in_=wv, reads=[ws], writes=[w])
                        else:
                            kb.op('dve', 'tensor_copy', out=w[:, k0:k0 + 2, :], in_=wv, reads=[ws], writes=[w])
                    ws3.append(w)
                wg, wu, wd = ws3
                cur_scatter = []
                for (c0, nch, rows) in ((0, 4, 128), (4, 4, 128), (8, 1, CAPC)):
                    N = nch * rows
                    xsT = xsTs.get()
                    for cc in range(nch):
                        c = c0 + cc
                        xs = xss.get()
                        ia = idx[:, e, c:c + 1] if c < 8 else idxc[:, e:e + 1]
                        irb = idx if c < 8 else idxc
                        def fng(eng, xs=xs, ia=ia, rows=rows):
                            return eng.indirect_dma_start(out=xs[0:rows, :], out_offset=None, in_=u2d,
                                                          in_offset=bass.IndirectOffsetOnAxis(ap=ia, axis=0),
                                                          bounds_check=self.breg(eng, NTOK - 1), oob_is_err=False)
                        kb._deps('pool', [irb], [])
                        kb.dma('pool', None, None, writes=[xs], fn=fng)
                        tp = pg.get()
                        tpv = tp[:].bitcast(BF16).rearrange("p (a b) -> p a b", b=128)
                        for kc in range(8):
                            kb.op('pe', 'transpose', out=tpv[:, kc, 0:rows], in_=xs[0:rows, kc * 128:(kc + 1) * 128], identity=self.idb[0:rows, 0:rows],
                                  reads=[xs, self.idb], writes=[tp], inc=(kc == 7))
                        kb.op('act', 'copy', out=xsT[:, :, cc * rows:(cc + 1) * rows], in_=tpv[:, 0:8, 0:rows], reads=[tp], writes=[xsT])
                    hT = hTs.get()
                    for f in range(8):
                        pG, pU = pg.get(), pg.get()
                        for kc in range(8):
                            kb.op('pe', 'matmul', out=pG[:, 0:N], lhsT=wg[:, kc, f * 128:(f + 1) * 128], rhs=xsT[:, kc, 0:N],
                                  start=(kc == 0), stop=(kc == 7), reads=[wg, xsT], writes=[pG], inc=(kc == 7))
                        for kc in range(8):
                            kb.op('pe', 'matmul', out=pU[:, 0:N], lhsT=wu[:, kc, f * 128:(f + 1) * 128], rhs=xsT[:, kc, 0:N],
                                  start=(kc == 0), stop=(kc == 7), reads=[wu, xsT], writes=[pU], inc=(kc == 7))
                        sg = sgs.get()
                        kb.op('act', 'activation', out=sg[:, 0:N], in_=pG[:, 0:N], func=AF.Silu, reads=[pG], writes=[sg])
                        kb.op('dve', 'tensor_tensor', out=hT[:, f, 0:N], in0=pU[:, 0:N], in1=sg[:, 0:N], op=ALU.mult, reads=[pU, sg], writes=[hT])
                    for cc in range(nch):
                        c = c0 + cc
                        yp = py.get()
                        for hf in range(2):
                            for f in range(8):
                                kb.op('pe', 'matmul', out=yp[0:rows, hf, :], lhsT=hT[:, f, cc * rows:(cc + 1) * rows], rhs=wd[:, f, hf * 512:(hf + 1) * 512],
                                      start=(f == 0), stop=(f == 7), reads=[hT, wd], writes=[yp], inc=(f == 7))
                        ysb = ysbs.get()
                        wa = L[:, e, c, 1:2] if c < 8 else Lc[:, e, 1:2]
                        wb_ = L if c < 8 else Lc
                        kb.op('dve', 'tensor_scalar', out=ysb[0:rows, :], in0=yp[0:rows].rearrange("p a b -> p (a b)"), scalar1=wa, scalar2=None,
                              op0=ALU.mult, reads=[yp, wb_], writes=[ysb])
                        ia = idx[:, e, c:c + 1] if c < 8 else idxc[:, e:e + 1]
                        def fns(eng, ysb=ysb, ia=ia, rows=rows):
                            return eng.indirect_dma_start(out=accd, out_offset=bass.IndirectOffsetOnAxis(ap=ia, axis=0),
                                                          in_=ysb[0:rows, :], in_offset=None, bounds_check=self.breg(eng, NTOK - 1), oob_is_err=True,
                                                          compute_op=ALU.add)
                        for (s_, v_) in prev_scatter:
                            kb.wait_dma('pool', s_, v_)
                        cur_scatter.append(kb.dma('pool', None, None, reads=[ysb], fn=fns))
                prev_scatter = cur_scatter
            kb.stage_end()

    def stageG(self, l, xout, lat_only):
        kb = self.kb
        with ExitStack() as st:
            sb = lambda shape, dt, name: kb.sb(shape, dt, name, stack=st)
            P2 = lambda f, n=2: Pool([f() for _ in range(n)])
            G2 = [self.modtile(st, r, 5, 'G2') for r in range(2)]
            LG = self.bcast_row(st, self.ln_g.ap()[l, 1], D, 'LG2')
            LBt = self.bcast_row(st, self.ln_b.ap()[l, 1], D, 'LB2')
            x1s = P2(lambda: sb([128, D], F32, 'x1g'), 3)
            acs = P2(lambda: sb([128, D], F32, 'acg'), 3)
            ots = P2(lambda: sb([128, D], F32, 'otg'), 3)
            stt = P2(lambda: sb([128, 2, 6], F32, 'sttg'))
            mvs = P2(lambda: sb([128, 2], F32, 'mvg'))
            rss = P2(lambda: sb([128, 1], F32, 'rsg'))
            nms = P2(lambda: sb([128, 1], F32, 'nmg'))
            for ti in range(2 if lat_only else 0, NT):
                r = 1 if ti < 2 else 0
                tok = slice(ti * 128, (ti + 1) * 128)
                x1, ac = x1s.get(), acs.get()
                kb.dma('sp', x1[:], self.x1.ap()[tok, :], writes=[x1])
                kb.dma('sp', ac[:], self.acc.ap()[tok, :], writes=[ac])
                kb.op('dve', 'tensor_tensor', out=ac[:], in0=ac[:], in1=G2[r][:], op=ALU.mult, reads=[ac, G2[r]], writes=[ac])
                kb.op('dve', 'scalar_tensor_tensor', out=ac[:], in0=x1[:], scalar=ALPHA, in1=ac[:], op0=ALU.mult, op1=ALU.add, reads=[x1, ac], writes=[ac])
                s6, mv, rs, nm = stt.get(), mvs.get(), rss.get(), nms.get()
                self.ln_stats(st, ac, rs, nm, s6, mv)
                ot = ots.get()
                kb.op('act', 'activation', out=ot[:], in_=ac[:], func=AF.Identity, bias=nm[:], scale=rs[:], reads=[ac, nm, rs], writes=[ot])
                kb.op('dve', 'tensor_tensor', out=ot[:], in0=ot[:], in1=LG[:], op=ALU.mult, reads=[ot, LG], writes=[ot])
                kb.op('dve', 'tensor_tensor', out=ot[:], in0=ot[:], in1=LBt[:], op=ALU.add, reads=[ot, LBt], writes=[ot])
                if lat_only:
                    kb.dma('pool', xout.ap()[(ti - 2) * 128:(ti - 1) * 128, :], ot[:], reads=[ot])
                else:
                    kb.dma('pool', xout.ap()[tok, :], ot[:], reads=[ot])
            kb.stage_end()

    def layer(self, l, xin, xout, last):
        self.stage0(l)
        self.stageA(l, xin)
        self.stageB(l, 0)
        self.stageB(l, 1)
        self.stageC(l)
        self.stageD(l, xin)
        self.stageE(l, first=(l == 0))
        self.stageF(l)
        self.stageG(l, xout, lat_only=last)


def build_program():
    P = Prog(debug=False)
    P.layer(0, P.x_all, P.xa, False)
    P.layer(1, P.xa, P.y, True)
    return P


_PROG = None


def kernel(x, c, ctx, c_ctx, w_mod, b_mod, w_in, attn_sink, hgrn_lb_fw, hgrn_lb_bw, hgrn_norm_g,
           w_branch_attn, w_branch_hgrn, w_out, w_router, w_gate, w_up, w_down, ln_g, ln_b):
    global _PROG
    f = lambda a: np.ascontiguousarray(np.asarray(a, dtype=np.float32))
    x, c, ctx, c_ctx = f(x), f(c), f(ctx), f(c_ctx)
    shared = dict(w_mod=f(w_mod), b_mod=f(b_mod), w_in=f(w_in), attn_sink=f(attn_sink), hgrn_lb_fw=f(hgrn_lb_fw),
                  hgrn_lb_bw=f(hgrn_lb_bw), hgrn_norm_g=f(hgrn_norm_g), w_branch_attn=f(w_branch_attn),
                  w_branch_hgrn=f(w_branch_hgrn), w_out=f(w_out), w_router=f(w_router), w_gate=f(w_gate), w_up=f(w_up),
                  w_down=f(w_down), ln_g=f(ln_g), ln_b=f(ln_b))
    cp, rope = make_consts()
    shared['cpack'] = cp
    shared['rope'] = rope
    B = x.shape[0]
    in_maps = []
    for b in range(B):
        d = dict(shared)
        d['x_all'] = np.ascontiguousarray(np.concatenate([ctx[b], x[b]], axis=0))
        d['c2'] = np.ascontiguousarray(np.stack([c[b], c_ctx], 0).reshape(16, 128))
        in_maps.append(d)
    if _PROG is None:
        _PROG = build_program()
    res = run_bass_kernel_spmd(_PROG.kb.nc, in_maps, core_ids=list(range(B)))
    return np.stack([np.asarray(r["y"], dtype=np.float32) for r in res.results], axis=0)
```

```python
import numpy as np
import ml_dtypes
from contextlib import ExitStack
import concourse.bass as bass
import concourse.mybir as mybir
from concourse.bass_utils import run_bass_kernel_spmd

F32 = mybir.dt.float32
BF16 = mybir.dt.bfloat16
I32 = mybir.dt.int32
U32 = mybir.dt.uint32
ALU = mybir.AluOpType
AF = mybir.ActivationFunctionType
AX = mybir.AxisListType

D = 1024
LCTX = 256
LSEQ = 8192
NT = (LCTX + LSEQ) // 128
NTOK = NT * 128
IN_DIM = 5376
NEXP = 16
CAP = 1024
CAPC = 32
LROW = CAP + CAPC
LN_EPS = 1e-6
ALPHA = 4.0 ** 0.25


class Buf:
    def __init__(self, kb, t, name):
        self.kb = kb
        self.t = t
        self.name = name
        self.w = None
        self.r = {}
        self.dr = 0
        self.dsem = None
        self.dcnt = 0

    def __getitem__(self, k):
        return self.t[k]

    def sem(self):
        if self.dsem is None:
            if self.kb.free_sems:
                self.dsem, self.dcnt = self.kb.free_sems.pop()
            else:
                self.dsem = self.kb.new_sem(self.name)
                self.dcnt = 0
        return self.dsem


class KB:
    ENG = ('pe', 'act', 'dve', 'pool', 'sp')

    def __init__(self):
        self.nc = bass.Bass("TRN2", target_bir_lowering=False)
        self.es = ExitStack()
        self.q = {e: [] for e in self.ENG}
        self.cnt = {e: 0 for e in self.ENG}
        self.pend = {e: False for e in self.ENG}
        self.known = {e: {} for e in self.ENG}
        self.nsem = 0
        self.psem = {e: self.new_sem('prog_' + e) for e in self.ENG}
        self.nbuf = 0
        self.relax_self = True
        self.inflight = {}
        self.free_sems = []
        self.stage_bufs = []

    def new_sem(self, name):
        self.nsem += 1
        return self.es.enter_context(self.nc.semaphore('s%d_%s' % (self.nsem, name)))

    def sb(self, shape, dt, name=None, stack=None):
        self.nbuf += 1
        name = '%s_%d' % (name or 'sb', self.nbuf)
        t = (stack or self.es).enter_context(self.nc.sbuf_tensor(name, list(shape), dt))
        b = Buf(self, t, name)
        if stack is not None:
            self.stage_bufs.append(b)
        return b

    def ps(self, shape, dt, name=None, stack=None):
        self.nbuf += 1
        name = '%s_%d' % (name or 'ps', self.nbuf)
        t = (stack or self.es).enter_context(self.nc.psum_tensor(name, list(shape), dt))
        b = Buf(self, t, name)
        if stack is not None:
            self.stage_bufs.append(b)
        return b

    def dram(self, name, shape, dt, kind="Internal"):
        return self.nc.dram_tensor(name, list(shape), dt, kind=kind)

    def _wait(self, eng, dep):
        if dep is None:
            return
        if dep[0] == 'e':
            _, e2, n = dep
            if e2 == 'pe' and eng == 'pe':
                return
            if e2 == eng and self.relax_self and n < self.cnt[eng]:
                return
            if self.known[eng].get(e2, 0) >= n:
                return
            self.known[eng][e2] = n
            self.q[eng].append(('w', self.psem[e2], n))
        else:
            _, sem, n = dep
            key = ('d', id(sem))
            if self.known[eng].get(key, 0) >= n:
                return
            self.known[eng][key] = n
            self.q[eng].append(('w', sem, n))

    def _deps(self, eng, reads, writes):
        for b in reads:
            self._wait(eng, b.w)
        for b in writes:
            self._wait(eng, b.w)
            for e2, n in b.r.items():
                if e2 != eng:
                    self._wait(eng, ('e', e2, n))
            if b.dr:
                self._wait(eng, ('d', b.sem(), b.dr))

    def op(self, eng, meth, reads=(), writes=(), inc=True, **kw):
        fn = (lambda e, m=meth, k=kw: getattr(e, m)(**k))
        self._deps(eng, reads, writes)
        n = self.cnt[eng] + 1
        if inc:
            self.cnt[eng] = n
            self.q[eng].append(('i', fn, self.psem[eng]))
            self.pend[eng] = False
        else:
            self.q[eng].append(('i', fn, None))
            self.pend[eng] = True
        for b in reads:
            b.r[eng] = n
        for b in writes:
            b.w = ('e', eng, n)
            b.r = {}
            b.dr = 0

    def dma(self, eng, out_ap, in_ap, reads=(), writes=(), fn=None):
        self._deps(eng, reads, writes)
        assert len(reads) + len(writes) >= 1
        tgt = (list(writes) + list(reads))[0]
        sem = tgt.sem()
        tgt.dcnt += 16
        val = tgt.dcnt
        if fn is None:
            fn = lambda e, o=out_ap, i=in_ap: e.dma_start(out=o, in_=i)
        self.q[eng].append(('d', fn, sem))
        self.inflight[id(sem)] = (sem, val)
        for b in writes:
            b.w = ('d', sem, val)
            b.r = {}
            b.dr = 0
        for b in reads:
            assert b is tgt or b.dsem is None or b.dsem is sem
            b.dsem = sem
            b.dcnt = max(b.dcnt, val)
            b.dr = val
        return sem, val

    def wait_dma(self, eng, sem, val):
        self._wait(eng, ('d', sem, val))

    def stage_end(self):
        for sem, val in self.inflight.values():
            self._wait('sp', ('d', sem, val))
        self.inflight = {}
        for e2 in self.ENG:
            if e2 != 'sp':
                self._wait('sp', ('e', e2, self.cnt[e2]))
        self.cnt['sp'] += 1
        self.q['sp'].append(('s', self.psem['sp']))
        for e in self.ENG:
            for e2 in self.ENG:
                if e2 != e:
                    self._wait(e, ('e', e2, self.cnt[e2]))
        self.flush()
        for b in self.stage_bufs:
            if b.dsem is not None:
                self.free_sems.append((b.dsem, b.dcnt))
                b.dsem = None
        self.stage_bufs = []

    def flush(self):
        nc = self.nc
        q = self.q
        with nc.Block() as block:
            def run(e, lst):
                for it in lst:
                    if it[0] == 'w':
                        e.wait_ge(it[1], it[2])
                    elif it[0] == 'i':
                        ins = it[1](e)
                        if it[2] is not None:
                            ins.then_inc(it[2], 1)
                    elif it[0] == 's':
                        e.sem_inc(it[1], 1)
                    else:
                        it[1](e).then_inc(it[2], 16)

            @block.tensor
            def _(e):
                run(e, q['pe'])

            @block.scalar
            def _(e):
                run(e, q['act'])

            @block.vector
            def _(e):
                run(e, q['dve'])

            @block.gpsimd
            def _(e):
                run(e, q['pool'])

            @block.sync
            def _(e):
                run(e, q['sp'])
        self.q = {e: [] for e in self.ENG}


class Pool:
    def __init__(self, bufs):
        self.bufs = bufs
        self.i = 0

    def get(self):
        b = self.bufs[self.i % len(self.bufs)]
        self.i += 1
        return b


C_ID, C_TIF, C_TIB, C_TSF, C_TSB, C_BD, C_OFF, C_IOTA, C_SEGB, C_EB, C_ONES, C_W = \
    0, 128, 256, 384, 512, 640, 768, 896, 1152, 1153, 1154, 1282


def make_consts():
    a = np.arange(128)[:, None]
    b = np.arange(128)[None, :]
    cp = np.zeros((128, C_W), np.float32)
    cp[:, C_ID:C_ID + 128] = (a == b)
    cp[:, C_TIF:C_TIF + 128] = (a <= b)
    cp[:, C_TIB:C_TIB + 128] = (a >= b)
    cp[:, C_TSF:C_TSF + 128] = (a > b)
    cp[:, C_TSB:C_TSB + 128] = (a < b)
    cp[:, C_BD:C_BD + 128] = (a // 8 == b // 8)
    cp[:, C_OFF:C_OFF + 128] = (a // 8 == b // 8) & (a % 8 < b % 8)
    cp[:, C_IOTA:C_IOTA + 256] = np.arange(256)[None, :]
    cp[:, C_SEGB] = (np.arange(128) % 8) * 1024 + LCTX
    cp[:, C_EB] = (np.arange(128) // 8) * LROW
    cp[:, C_ONES:C_ONES + 128] = 1.0
    rope = np.zeros((NTOK, 64), np.float32)
    rope[:, :32] = 1.0
    t = np.arange(LSEQ)
    row = (t // 64).astype(np.float32)
    col = (t % 64).astype(np.float32)
    inv = (10000.0 ** (-np.arange(16, dtype=np.float32) / 16)).astype(np.float32)
    ar = row[:, None] * inv[None, :]
    ac = col[:, None] * inv[None, :]
    rope[LCTX:, 0:16] = np.cos(ar)
    rope[LCTX:, 16:32] = np.cos(ac)
    rope[LCTX:, 32:48] = np.sin(ar)
    rope[LCTX:, 48:64] = np.sin(ac)
    return cp, rope


def groups():
    g = [(0, 2)]
    for s in range(2, NT, 4):
        g.append((s, 4))
    return g


class Prog:
    def __init__(self, debug=False):
        self.kb = kb = KB()
        self.debug = debug
        E = "ExternalInput"
        d = kb.dram
        self.x_all = d("x_all", [NTOK, D], F32, E)
        self.c2 = d("c2", [16, 128], F32, E)
        self.cpack = d("cpack", [128, C_W], F32, E)
        self.rope = d("rope", [NTOK, 64], F32, E)
        self.w_mod = d("w_mod", [2, D, 6 * D], F32, E)
        self.b_mod = d("b_mod", [2, 6 * D], F32, E)
        self.w_in = d("w_in", [2, D, IN_DIM], F32, E)
        self.attn_sink = d("attn_sink", [2, 8], F32, E)
        self.lb_fw = d("hgrn_lb_fw", [2, 512], F32, E)
        self.lb_bw = d("hgrn_lb_bw", [2, 512], F32, E)
        self.norm_g = d("hgrn_norm_g", [2, 512], F32, E)
        self.w_ba = d("w_branch_attn", [2, 512, D], F32, E)
        self.w_bh = d("w_branch_hgrn", [2, 512, D], F32, E)
        self.w_out = d("w_out", [2, D, D], F32, E)
        self.w_router = d("w_router", [2, D, NEXP], F32, E)
        self.w_gate = d("w_gate", [2, NEXP, D, D], F32, E)
        self.w_up = d("w_up", [2, NEXP, D, D], F32, E)
        self.w_down = d("w_down", [2, NEXP, D, D], F32, E)
        self.ln_g = d("ln_g", [2, 2, D], F32, E)
        self.ln_b = d("ln_b", [2, 2, D], F32, E)
        self.y = d("y", [LSEQ, D], F32, "ExternalOutput")
        dbg = debug
        self.modrow = d("modrow", [2, 6 * D], F32, "ExternalOutput" if (dbg is True or (dbg and "modrow" in dbg)) else "Internal")
        self.qT = d("s_qT", [512, NTOK], BF16, "ExternalOutput" if (dbg is True or (dbg and "s_qT" in dbg)) else "Internal")
        self.kT = d("s_kT", [128, NTOK], BF16, "ExternalOutput" if (dbg is True or (dbg and "s_kT" in dbg)) else "Internal")
        self.va = d("s_va", [NTOK, 128], BF16, "ExternalOutput" if (dbg is True or (dbg and "s_va" in dbg)) else "Internal")
        self.qhT = d("s_qhT", [512, NTOK], BF16, "ExternalOutput" if (dbg is True or (dbg and "s_qhT" in dbg)) else "Internal")
        self.lf = d("s_lf", [NTOK, 1024], F32, "ExternalOutput" if (dbg is True or (dbg and "s_lf" in dbg)) else "Internal")
        self.kk = d("s_kk", [NTOK, 1024], BF16, "ExternalOutput" if (dbg is True or (dbg and "s_kk" in dbg)) else "Internal")
        self.kkT = d("s_kkT", [1024, NTOK], BF16, "ExternalOutput" if (dbg is True or (dbg and "s_kkT" in dbg)) else "Internal")
        self.ih = d("s_ih", [NTOK, 512], BF16, "ExternalOutput" if (dbg is True or (dbg and "s_ih" in dbg)) else "Internal")
        self.sgh = d("s_sgh", [NTOK, 512], BF16, "ExternalOutput" if (dbg is True or (dbg and "s_sgh" in dbg)) else "Internal")
        self.sgaT = d("s_sgaT", [1024, NTOK], BF16, "ExternalOutput" if (dbg is True or (dbg and "s_sgaT" in dbg)) else "Internal")
        self.sgbT = d("s_sgbT", [1024, NTOK], BF16, "ExternalOutput" if (dbg is True or (dbg and "s_sgbT" in dbg)) else "Internal")
        self.ofw = d("s_ofw", [NTOK, 512], F32, "ExternalOutput" if (dbg is True or (dbg and "s_ofw" in dbg)) else "Internal")
        self.hgT = d("s_hgT", [512, NTOK], BF16, "ExternalOutput" if (dbg is True or (dbg and "s_hgT" in dbg)) else "Internal")
        self.attnT = d("s_attnT", [512, NTOK], BF16, "ExternalOutput" if (dbg is True or (dbg and "s_attnT" in dbg)) else "Internal")
        self.x1 = d("s_x1", [NTOK, D], F32, "ExternalOutput" if (dbg is True or (dbg and "s_x1" in dbg)) else "Internal")
        self.u2 = d("s_u2", [NTOK, D], BF16, "ExternalOutput" if (dbg is True or (dbg and "s_u2" in dbg)) else "Internal")
        self.affT = d("s_affT", [NEXP, NTOK], F32, "ExternalOutput" if (dbg is True or (dbg and "s_affT" in dbg)) else "Internal")
        self.pairs = d("s_pairs", [NEXP * LROW + 256, 2], F32, "ExternalOutput" if (dbg is True or (dbg and "s_pairs" in dbg)) else "Internal")
        self.acc = d("s_acc", [NTOK, D], F32, "ExternalOutput" if (dbg is True or (dbg and "s_acc" in dbg)) else "Internal")
        self.xa = d("s_xa", [NTOK, D], F32, "ExternalOutput" if (dbg is True or (dbg and "s_xa" in dbg)) else "Internal")
        self.cp = kb.sb([128, C_W], F32, 'cp')
        self.idb = kb.sb([128, 128], BF16, 'idb')
        self.mfw = kb.sb([128, 128], BF16, 'mfw')
        self.mbw = kb.sb([128, 128], BF16, 'mbw')
        kb.dma('sp', self.cp[:], self.cpack.ap(), writes=[self.cp])
        kb.op('dve', 'tensor_copy', out=self.idb[:], in_=self.cp[:, C_ID:C_ID + 128], reads=[self.cp], writes=[self.idb])
        kb.op('dve', 'tensor_copy', out=self.mfw[:], in_=self.cp[:, C_TIF:C_TIF + 128], reads=[self.cp], writes=[self.mfw])
        kb.op('dve', 'tensor_copy', out=self.mbw[:], in_=self.cp[:, C_TIB:C_TIB + 128], reads=[self.cp], writes=[self.mbw])
        self.ngp = kb.sb([128, 128], BF16, 'ngp')
        self.ngn = kb.sb([128, 128], BF16, 'ngn')
        kb.op('dve', 'tensor_scalar', out=self.ngp[:], in0=self.cp[:, C_TIB:C_TIB + 128], scalar1=30000.0, scalar2=-30000.0,
              op0=ALU.mult, op1=ALU.add, reads=[self.cp], writes=[self.ngp])
        kb.op('dve', 'tensor_scalar', out=self.ngn[:], in0=self.cp[:, C_TIF:C_TIF + 128], scalar1=30000.0, scalar2=-30000.0,
              op0=ALU.mult, op1=ALU.add, reads=[self.cp], writes=[self.ngn])
        kb.stage_end()

    def breg(self, eng, val):
        if not hasattr(self, '_bregs'):
            self._bregs = {}
        if val not in self._bregs:
            self._bregs[val] = eng.to_reg(val)
        return self._bregs[val]

    def ln_stats(self, st, src, rs, nm, stt, mv):
        kb = self.kb
        kb.op('dve', 'bn_stats', out=stt[:, 0, :], in_=src[:, 0:512], reads=[src], writes=[stt])
        kb.op('dve', 'bn_stats', out=stt[:, 1, :], in_=src[:, 512:1024], reads=[src], writes=[stt])
        kb.op('dve', 'bn_aggr', out=mv[:], in_=stt[:].rearrange("p a b -> p (a b)"), reads=[stt], writes=[mv])
        kb.op('act', 'activation', out=rs[:], in_=mv[:, 1:2], func=AF.Sqrt, bias=LN_EPS, scale=1.0,
              reads=[mv], writes=[rs])
        kb.op('dve', 'reciprocal', out=rs[:], in_=rs[:], reads=[rs], writes=[rs])
        kb.op('pool', 'tensor_scalar', out=nm[:], in0=mv[:, 0:1], scalar1=rs[:], scalar2=-1.0,
              op0=ALU.mult, op1=ALU.mult, reads=[mv, rs], writes=[nm])

    def bcast_row(self, st, dram_ap_1d, n, name):
        kb = self.kb
        t = kb.sb([128, n], F32, name, stack=st)
        kb.dma('sp', t[:], dram_ap_1d.partition_broadcast(128), writes=[t])
        return t

    def mk_eps(self, st):
        kb = self.kb
        self.eps = kb.sb([128, 1], F32, 'eps', stack=st)
        kb.op('pool', 'memset', ap=self.eps[:], constant=LN_EPS, writes=[self.eps])

    def stage0(self, l):
        kb = self.kb
        with ExitStack() as st:
            c2s = kb.sb([16, 128], F32, 'c2s', stack=st)
            kb.dma('sp', c2s[:], self.c2.ap(), writes=[c2s])
            tp = kb.ps([128, 16], F32, 'tp0', stack=st)
            kb.op('pe', 'transpose', out=tp[:], in_=c2s[:], identity=self.cp[0:16, C_ID:C_ID + 16], reads=[c2s, self.cp], writes=[tp])
            sil = kb.sb([128, 2, 8], F32, 'sil', stack=st)
            kb.op('act', 'activation', out=sil[:].rearrange("p a b -> p (a b)"), in_=tp[:], func=AF.Silu, reads=[tp], writes=[sil])
            bm = kb.sb([2, 6 * D], F32, 'bm', stack=st)
            kb.dma('sp', bm[:], self.b_mod.ap()[l].partition_broadcast(2), writes=[bm])
            mod = kb.sb([2, 6 * D], F32, 'mod', stack=st)
            wms = Pool([kb.sb([128, 8, 512], F32, 'wm', stack=st) for _ in range(2)])
            pps = Pool([kb.ps([2, 512], F32, 'pm', stack=st) for _ in range(2)])
            for n in range(12):
                wm = wms.get()
                kb.dma('sp', wm[:], self.w_mod.ap()[l][:, n * 512:(n + 1) * 512].rearrange("(kc k) n -> k kc n", k=128), writes=[wm])
                pm = pps.get()
                for kc in range(8):
                    kb.op('pe', 'matmul', out=pm[:], lhsT=sil[:, :, kc], rhs=wm[:, kc, :], start=(kc == 0), stop=(kc == 7),
                          reads=[sil, wm], writes=[pm], inc=(kc == 7))
                kb.op('dve', 'tensor_tensor', out=mod[:, n * 512:(n + 1) * 512], in0=pm[:], in1=bm[:, n * 512:(n + 1) * 512], op=ALU.add,
                      reads=[pm, bm], writes=[mod])
            for c0 in (1024, 4096):
                kb.op('dve', 'tensor_scalar_add', out=mod[:, c0:c0 + 1024], in0=mod[:, c0:c0 + 1024], scalar1=1.0, reads=[mod], writes=[mod])
            kb.dma('sp', self.modrow.ap(), mod[:], reads=[mod])
            kb.stage_end()

    def modtile(self, st, r, k, name):
        return self.bcast_row(st, self.modrow.ap()[r, k * D:(k + 1) * D], D, name)

    def stageA(self, l, xin):
        kb = self.kb
        cp = self.cp
        with ExitStack() as st:
            sb = lambda shape, dt, name: kb.sb(shape, dt, name, stack=st)
            W = sb([128, 8, IN_DIM], BF16, 'W')
            wst = Pool([sb([128, 8, 256], F32, 'wst') for _ in range(2)])
            for n in range(IN_DIM // 256):
                ws = wst.get()
                kb.dma('sp', ws[:], self.w_in.ap()[l][:, n * 256:(n + 1) * 256].rearrange("(kc k) n -> k kc n", k=128), writes=[ws])
                kb.op('dve' if n % 2 else 'act', 'tensor_copy' if n % 2 else 'copy', out=W[:, :, n * 256:(n + 1) * 256], in_=ws[:], reads=[ws], writes=[W])
            A1 = [self.modtile(st, r, 1, 'A1') for r in range(2)]
            B1 = [self.modtile(st, r, 0, 'B1') for r in range(2)]
            if l == 1:
                lbl = sb([128, 2, 2, 512], F32, 'lbl')
                kb.dma('sp', lbl[:, 0], self.lb_fw.ap().partition_broadcast(128), writes=[lbl])
                kb.dma('sp', lbl[:, 1], self.lb_bw.ap().partition_broadcast(128), writes=[lbl])
                LB = sb([128, 2, 512], F32, 'LB')
                OML = sb([128, 2, 512], F32, 'OML')
                kb.op('dve', 'tensor_tensor', out=LB[:], in0=lbl[:, :, 1, :], in1=lbl[:, :, 0, :], op=ALU.subtract, reads=[lbl], writes=[LB])
                kb.op('act', 'activation', out=LB[:], in_=LB[:], func=AF.Sigmoid, reads=[LB], writes=[LB])
                kb.op('dve', 'tensor_scalar', out=OML[:], in0=LB[:], scalar1=-1.0, scalar2=1.0, op0=ALU.mult, op1=ALU.add, reads=[LB], writes=[OML])
            xts = Pool([sb([128, D], F32, 'xt') for _ in range(2)])
            xns = Pool([sb([128, D], F32, 'xn') for _ in range(1)])
            us = Pool([sb([128, D], BF16, 'u') for _ in range(1)])
            uTs = Pool([sb([128, 8, 512], BF16, 'uT') for _ in range(2)])
            stt = Pool([sb([128, 2, 6], F32, 'stt') for _ in range(2)])
            mvs = Pool([sb([128, 2], F32, 'mv') for _ in range(2)])
            rss = Pool([sb([128, 1], F32, 'rs') for _ in range(2)])
            nms = Pool([sb([128, 1], F32, 'nm') for _ in range(2)])
            rts = Pool([sb([128, 64], F32, 'rt') for _ in range(2)])
            fos = Pool([sb([128, 512], BF16, 'fo') for _ in range(3)])
            qrs = Pool([sb([128, 640], BF16, 'qr') for _ in range(2)])
            tmpq = [sb([128, 256], F32, 'tmpq%d' % i) for i in range(4)]
            tmpk = [sb([128, 64], F32, 'tmpk%d' % i) for i in range(4)]
            ksbs = Pool([sb([128, 128], F32, 'ksb') for _ in range(2)])
            qkTs = Pool([sb([128, 5, 128], BF16, 'qkT') for _ in range(2)])
            vas = Pool([sb([128, 128], BF16, 'va') for _ in range(2)])
            sgs = Pool([sb([128, 1024], F32, 'sg') for _ in range(1)])
            lfs = Pool([sb([128, 1024], F32, 'lfo') for _ in range(1)])
            kks = Pool([sb([128, 1024], BF16, 'kko') for _ in range(2)])
            kkTs = Pool([sb([128, 8, 128], BF16, 'kkTo') for _ in range(2)])
            ihs = Pool([sb([128, 512], BF16, 'iho') for _ in range(2)])
            ghs = Pool([sb([128, 512], BF16, 'gho') for _ in range(2)])
            pp = Pool([kb.ps([128, 512], F32, 'ppA', stack=st) for _ in range(8)])

            def proj_tok(uT, j, c0, ncols):
                p = pp.get()
                for kc in range(8):
                    kb.op('pe', 'matmul', out=p[:, 0:ncols], lhsT=uT[:, kc, j * 128:(j + 1) * 128], rhs=W[:, kc, c0:c0 + ncols],
                          start=(kc == 0), stop=(kc == 7), reads=[uT, W], writes=[p], inc=(kc == 7))
                return p

            def tr_bf(src_ap, src_buf, nblk):
                p = pp.get()
                pv = p[:].bitcast(BF16).rearrange("p (a b) -> p a b", b=128)
                for k in range(nblk):
                    kb.op('pe', 'transpose', out=pv[:, k, :], in_=src_ap[:, k * 128:(k + 1) * 128], identity=self.idb[:],
                          reads=[src_buf, self.idb], writes=[p], inc=(k == nblk - 1))
                return p, pv

            for (t0, nt) in groups():
                N = nt * 128
                r = 1 if t0 < 2 else 0
                uT = uTs.get()
                for j in range(nt):
                    ti = t0 + j
                    xt = xts.get()
                    kb.dma('sp', xt[:], xin.ap()[ti * 128:(ti + 1) * 128, :], writes=[xt])
                    s6, mv, rs, nm = stt.get(), mvs.get(), rss.get(), nms.get()
                    self.ln_stats(st, xt, rs, nm, s6, mv)
                    xn = xns.get()
                    kb.op('act', 'activation', out=xn[:], in_=xt[:], func=AF.Identity, bias=nm[:], scale=rs[:], reads=[xt, nm, rs], writes=[xn])
                    kb.op('dve', 'tensor_tensor', out=xn[:], in0=xn[:], in1=A1[r][:], op=ALU.mult, reads=[xn, A1[r]], writes=[xn])
                    u = us.get()
                    kb.op('dve', 'tensor_tensor', out=u[:], in0=xn[:], in1=B1[r][:], op=ALU.add, reads=[xn, B1[r]], writes=[u])
                    p, pv = tr_bf(u[:], u, 8)
                    kb.op('act', 'copy', out=uT[:, :, j * 128:(j + 1) * 128], in_=pv[:, 0:8, :], reads=[p], writes=[uT])
                for (c0, nch, dst, fn) in ((768, 4, self.qhT, AF.Silu), (3328, 8, self.sgaT, AF.Sigmoid), (4352, 8, self.sgbT, AF.Sigmoid)):
                    for n in range(nch):
                        p = pp.get()
                        for kc in range(8):
                            kb.op('pe', 'matmul', out=p[:, 0:N], lhsT=W[:, kc, c0 + n * 128:c0 + (n + 1) * 128], rhs=uT[:, kc, 0:N],
                                  start=(kc == 0), stop=(kc == 7), reads=[uT, W], writes=[p], inc=(kc == 7))
                        fo = fos.get()
                        kb.op('act', 'activation', out=fo[:, 0:N], in_=p[:, 0:N], func=fn, reads=[p], writes=[fo])
                        kb.dma('pool', dst.ap()[n * 128:(n + 1) * 128, t0 * 128:t0 * 128 + N], fo[:, 0:N], reads=[fo])
                for j in range(nt):
                    ti = t0 + j
                    tok = slice(ti * 128, (ti + 1) * 128)
                    rt = rts.get()
                    kb.dma('sp', rt[:], self.rope.ap()[tok, :], writes=[rt])
                    pq = proj_tok(uT, j, 0, 512)
                    pk = proj_tok(uT, j, 512, 256)
                    qr = qrs.get()
                    ksb = ksbs.get()
                    kb.op('act', 'copy', out=ksb[:], in_=pk[:, 0:128], reads=[pk], writes=[ksb])
                    for (src, H, o0, eng, tmp) in ((pq, 8, 0, 'dve', tmpq), (ksb, 2, 512, 'pool', tmpk)):
                        sv = src[:, 0:H * 64].rearrange("p (h a b c) -> p h a b c", h=H, a=2, b=2)
                        x1, x2 = sv[:, :, :, 0, :], sv[:, :, :, 1, :]
                        cosb = rt[:, 0:32].rearrange("p (a c) -> p a c", a=2).unsqueeze(1).to_broadcast([128, H, 2, 16])
                        sinb = rt[:, 32:64].rearrange("p (a c) -> p a c", a=2).unsqueeze(1).to_broadcast([128, H, 2, 16])
                        tv = [t[:, 0:H * 32].rearrange("p (h a c) -> p h a c", h=H, a=2) for t in tmp]
                        ov = qr[:, o0:o0 + H * 64].rearrange("p (h a b c) -> p h a b c", h=H, a=2, b=2)
                        kb.op(eng, 'tensor_tensor', out=tv[0], in0=x1, in1=cosb, op=ALU.mult, reads=[src, rt], writes=[tmp[0]])
                        kb.op(eng, 'tensor_tensor', out=tv[1], in0=x2, in1=sinb, op=ALU.mult, reads=[src, rt], writes=[tmp[1]])
                        kb.op(eng, 'tensor_tensor', out=tv[2], in0=x2, in1=cosb, op=ALU.mult, reads=[src, rt], writes=[tmp[2]])
                        kb.op(eng, 'tensor_tensor', out=tv[3], in0=x1, in1=sinb, op=ALU.mult, reads=[src, rt], writes=[tmp[3]])
                        kb.op(eng, 'tensor_tensor', out=ov[:, :, :, 0, :], in0=tv[0], in1=tv[1], op=ALU.subtract, reads=[tmp[0], tmp[1]], writes=[qr])
                        kb.op(eng, 'tensor_tensor', out=ov[:, :, :, 1, :], in0=tv[2], in1=tv[3], op=ALU.add, reads=[tmp[2], tmp[3]], writes=[qr])
                    vo = vas.get()
                    kb.op('act', 'copy', out=vo[:], in_=pk[:, 128:256], reads=[pk], writes=[vo])
                    kb.dma('pool', self.va.ap()[tok, :], vo[:], reads=[vo])
                    p, pv = tr_bf(qr[:], qr, 5)
                    qkT = qkTs.get()
                    kb.op('act', 'copy', out=qkT[:], in_=pv[:, 0:5, :], reads=[p], writes=[qkT])
                    kb.dma('pool', self.qT.ap()[:, tok].rearrange("(j p) t -> p j t", p=128), qkT[:, 0:4, :], reads=[qkT])
                    kb.dma('pool', self.kT.ap()[:, tok], qkT[:, 4, :], reads=[qkT])
                    pzf = proj_tok(uT, j, 1280, 512)
                    pzb = proj_tok(uT, j, 1792, 512)
                    sg = sgs.get()
                    kb.op('act', 'activation', out=sg[:, 0:512], in_=pzf[:], func=AF.Sigmoid, reads=[pzf], writes=[sg])
                    kb.op('act', 'activation', out=sg[:, 512:1024], in_=pzb[:], func=AF.Sigmoid, reads=[pzb], writes=[sg])
                    if l == 1:
                        kb.op('dve', 'tensor_tensor', out=sg[:], in0=sg[:], in1=OML[:].rearrange("p a b -> p (a b)"), op=ALU.mult, reads=[sg, OML], writes=[sg])
                        kb.op('dve', 'tensor_tensor', out=sg[:], in0=sg[:], in1=LB[:].rearrange("p a b -> p (a b)"), op=ALU.add, reads=[sg, LB], writes=[sg])
                    lfo = lfs.get()
                    kb.op('act', 'activation', out=lfo[:], in_=sg[:], func=AF.Ln, reads=[sg], writes=[lfo])
                    kb.dma('pool', self.lf.ap()[tok, :], lfo[:], reads=[lfo])
                    kko = kks.get()
                    kb.op('dve', 'tensor_scalar', out=kko[:], in0=sg[:], scalar1=-1.0, scalar2=1.0, op0=ALU.mult, op1=ALU.add, reads=[sg], writes=[kko])
                    kb.dma('pool', self.kk.ap()[tok, :], kko[:], reads=[kko])
                    p, pv = tr_bf(kko[:], kko, 8)
                    kkTo = kkTs.get()
                    kb.op('act', 'copy', out=kkTo[:], in_=pv[:, 0:8, :], reads=[p], writes=[kkTo])
                    kb.dma('pool', self.kkT.ap()[:, tok].rearrange("(j p) t -> p j t", p=128), kkTo[:], reads=[kkTo])
                    pih = proj_tok(uT, j, 2304, 512)
                    iho = ihs.get()
                    kb.op('dve', 'tensor_copy', out=iho[:], in_=pih[:], reads=[pih], writes=[iho])
                    kb.dma('pool', self.ih.ap()[tok, :], iho[:], reads=[iho])
                    pgh = proj_tok(uT, j, 2816, 512)
                    gho = ghs.get()
                    kb.op('act', 'activation', out=gho[:], in_=pgh[:], func=AF.Silu, reads=[pgh], writes=[gho])
                    kb.dma('pool', self.sgh.ap()[tok, :], gho[:], reads=[gho])
            kb.stage_end()

    def stageB(self, l, dr):
        kb = self.kb
        cp = self.cp
        with ExitStack() as st:
            sb = lambda shape, dt, name: kb.sb(shape, dt, name, stack=st)
            P2 = lambda f, n=2: Pool([f() for _ in range(n)])
            TI = cp[:, (C_TIF if dr == 0 else C_TIB):(C_TIF if dr == 0 else C_TIB) + 128]
            TS = cp[:, (C_TSF if dr == 0 else C_TSB):(C_TSF if dr == 0 else C_TSB) + 128]
            MK = self.mfw if dr == 0 else self.mbw
            S = sb([128, 4, 128], F32, 'S')
            Sb = sb([128, 4, 128], BF16, 'Sb')
            kb.op('dve', 'memset', ap=S[:], constant=0.0, writes=[S])
            kb.op('dve', 'memset', ap=Sb[:], constant=0.0, writes=[Sb])
            lfs = P2(lambda: sb([128, 512], F32, 'lf'))
            kks = P2(lambda: sb([128, 512], BF16, 'kk'))
            kkTs = P2(lambda: sb([128, 4, 128], BF16, 'kkT'))
            qTs = P2(lambda: sb([128, 4, 128], BF16, 'qT'))
            ihs = P2(lambda: sb([128, 512], BF16, 'ih'))
            cumSs = P2(lambda: sb([128, 4, 128], F32, 'cumS'))
            eEs = P2(lambda: sb([128, 512], F32, 'eE'))
            khats = P2(lambda: sb([128, 512], BF16, 'khat'))
            Rqs = P2(lambda: sb([128, 4, 4], F32, 'Rq'))
            for rq in Rqs.bufs:
                kb.op('dve', 'memset', ap=rq[:], constant=0.0, writes=[rq])
            argqs = P2(lambda: sb([128, 4, 128], F32, 'argq'))
            qts = P2(lambda: sb([128, 4, 128], BF16, 'qt'))
            eSs = P2(lambda: sb([128, 4, 128], F32, 'eS'))
            qSs = P2(lambda: sb([128, 4, 128], BF16, 'qS'))
            eqs = P2(lambda: sb([128, 4, 128], F32, 'eq'))
            eGs = P2(lambda: sb([128, 4, 128], F32, 'eG'))
            KGs = P2(lambda: sb([128, 4, 128], BF16, 'KG'))
            Ffs = P2(lambda: sb([128, 4, 4, 4], F32, 'Ff'))
            NEG = sb([128, 4, 4], F32, 'NEG')
            kb.op('dve', 'memset', ap=NEG[:], constant=0.0, writes=[NEG])
            for a_ in range(4):
                for b_ in range(4):
                    if (b_ > a_) if dr == 0 else (b_ < a_):
                        kb.op('dve', 'memset', ap=NEG[:, a_, b_:b_ + 1], constant=-30000.0, writes=[NEG])
            Kts = P2(lambda: sb([128, 4, 4, 128], BF16, 'Kt'))
            scTs = P2(lambda: sb([128, 4, 128], BF16, 'scT'))
            etots = P2(lambda: sb([128, 4], F32, 'etot'))
            osbs = P2(lambda: sb([128, 4, 128], F32, 'osb'))
            pp = Pool([kb.ps([128, 512], F32, 'ppB', stack=st) for _ in range(8)])
            if dr == 1:
                ofws = P2(lambda: sb([128, 512], F32, 'ofw'))
                sghs = P2(lambda: sb([128, 512], BF16, 'sgh'))
                NG = self.bcast_row(st, self.norm_g.ap()[l], 512, 'NG')
                sqs = P2(lambda: sb([128, 128], F32, 'sq'))
                sss = P2(lambda: sb([128, 4], F32, 'ss'))
                hgs = P2(lambda: sb([128, 4, 128], F32, 'hgf'))
                hgbs = P2(lambda: sb([128, 512], BF16, 'hgb'))
                hgTs = P2(lambda: sb([128, 4, 128], BF16, 'hgT'))
            order = list(range(NT)) if dr == 0 else [1, 0] + list(range(NT - 1, 1, -1))
            last = 127 if dr == 0 else 0
            for ti in order:
                tok = slice(ti * 128, (ti + 1) * 128)
                cs = slice(dr * 512, (dr + 1) * 512)
                lf, kk, kkT, qT, ih = lfs.get(), kks.get(), kkTs.get(), qTs.get(), ihs.get()
                kb.dma('sp', lf[:], self.lf.ap()[tok, cs], writes=[lf])
                kb.dma('sp', kk[:], self.kk.ap()[tok, cs], writes=[kk])
                kb.dma('sp', kkT[:], self.kkT.ap()[cs, tok].rearrange("(h k) t -> k h t", k=128), writes=[kkT])
                kb.dma('sp', qT[:], self.qhT.ap()[:, tok].rearrange("(h k) t -> k h t", k=128), writes=[qT])
                kb.dma('sp', ih[:], self.ih.ap()[tok, :], writes=[ih])
                if dr == 1:
                    ofw, sgh = ofws.get(), sghs.get()
                    kb.dma('sp', ofw[:], self.ofw.ap()[tok, :], writes=[ofw])
                    kb.dma('sp', sgh[:], self.sgh.ap()[tok, :], writes=[sgh])
                cps = pp.get()
                cpv = cps[:].rearrange("p (h t) -> p h t", h=4)
                for h in range(4):
                    kb.op('pe', 'matmul', out=cpv[:, h, :], lhsT=lf[:, h * 128:(h + 1) * 128], rhs=TI, start=True, stop=True,
                          reads=[lf, cp], writes=[cps], inc=(h == 3))
                eps_ = pp.get()
                kb.op('pe', 'matmul', out=eps_[:], lhsT=TS, rhs=lf[:], start=True, stop=True, reads=[lf, cp], writes=[eps_])
                cumS = cumSs.get()
                kb.op('act', 'copy', out=cumS[:], in_=cpv, reads=[cps], writes=[cumS])
                eE = eEs.get()
                kb.op('act', 'activation', out=eE[:], in_=eps_[:], func=AF.Exp, reads=[eps_], writes=[eE])
                khat = khats.get()
                kb.op('dve', 'tensor_tensor', out=khat[:], in0=kk[:], in1=eE[:], op=ALU.mult, reads=[kk, eE], writes=[khat])
                Rq = Rqs.get()
                c4 = cumS[:].rearrange("p h (a c) -> p h a c", a=4)
                if dr == 0:
                    kb.op('pool', 'tensor_copy', out=Rq[:, :, 1:4], in_=c4[:, :, 0:3, 31], reads=[cumS], writes=[Rq])
                else:
                    kb.op('pool', 'tensor_copy', out=Rq[:, :, 0:3], in_=c4[:, :, 1:4, 0], reads=[cumS], writes=[Rq])
                argq = argqs.get()
                kb.op('dve', 'tensor_tensor', out=argq[:].rearrange("p h (a c) -> p h a c", a=4), in0=c4,
                      in1=Rq[:].unsqueeze(3).to_broadcast([128, 4, 4, 32]), op=ALU.subtract, reads=[cumS, Rq], writes=[argq])
                kb.op('dve', 'tensor_scalar_max', out=argq[:], in0=argq[:], scalar1=-69.0, reads=[argq], writes=[argq])
                eq = eqs.get()
                kb.op('act', 'activation', out=eq[:], in_=argq[:], func=AF.Exp, reads=[argq], writes=[eq])
                qt = qts.get()
                kb.op('dve', 'tensor_tensor', out=qt[:], in0=eq[:], in1=qT[:], op=ALU.mult, reads=[eq, qT], writes=[qt])
                eG = eGs.get()
                kb.op('act', 'activation', out=eG[:], in_=argq[:], func=AF.Exp, scale=-1.0, reads=[argq], writes=[eG])
                KG = KGs.get()
                kb.op('dve', 'tensor_tensor', out=KG[:], in0=eG[:], in1=kkT[:], op=ALU.mult, reads=[eG, kkT], writes=[KG])
                eS = eSs.get()
                kb.op('act', 'activation', out=eS[:], in_=cumS[:], func=AF.Exp, reads=[cumS], writes=[eS])
                qS = qSs.get()
                kb.op('dve', 'tensor_tensor', out=qS[:], in0=eS[:], in1=qT[:], op=ALU.mult, reads=[eS, qT], writes=[qS])
                Ff = Ffs.get()
                kb.op('pool', 'tensor_tensor', out=Ff[:], in0=Rq[:].unsqueeze(3).to_broadcast([128, 4, 4, 4]),
                      in1=Rq[:].unsqueeze(2).to_broadcast([128, 4, 4, 4]), op=ALU.subtract, reads=[Rq], writes=[Ff])
                kb.op('pool', 'tensor_tensor', out=Ff[:], in0=Ff[:], in1=NEG[:].unsqueeze(1).to_broadcast([128, 4, 4, 4]), op=ALU.add,
                      reads=[Ff, NEG], writes=[Ff])
                kb.op('act', 'activation', out=Ff[:], in_=Ff[:], func=AF.Exp, reads=[Ff], writes=[Ff])
                Kt = Kts.get()
                for h in range(4):
                    kb.op('dve', 'tensor_tensor', out=Kt[:, h].rearrange("p a (b c) -> p a b c", b=4),
                          in0=KG[:, h, :].rearrange("p (b c) -> p b c", b=4).unsqueeze(1).to_broadcast([128, 4, 4, 32]),
                          in1=Ff[:, h].unsqueeze(3).to_broadcast([128, 4, 4, 32]), op=ALU.mult, reads=[KG, Ff], writes=[Kt])
                sps = pp.get()
                spv = sps[:].rearrange("p (h t) -> p h t", h=4)
                for h in range(4):
                    for a in range(4):
                        kb.op('pe', 'matmul', out=spv[:, h, a * 32:(a + 1) * 32], lhsT=Kt[:, h, a, :], rhs=qt[:, h, a * 32:(a + 1) * 32],
                              start=True, stop=True, reads=[Kt, qt], writes=[sps], inc=(h == 3 and a == 3))
                scT = scTs.get()
                kb.op('dve', 'tensor_tensor', out=scT[:], in0=spv, in1=MK[:].unsqueeze(1).to_broadcast([128, 4, 128]), op=ALU.mult,
                      reads=[sps, MK], writes=[scT])
                ops_ = pp.get()
                opv = ops_[:].rearrange("p (h t) -> p h t", h=4)
                for h in range(4):
                    kb.op('pe', 'matmul', out=opv[:, h, :], lhsT=qS[:, h, :], rhs=Sb[:, h, :], start=True, stop=False,
                          reads=[qS, Sb], writes=[ops_], inc=False)
                    kb.op('pe', 'matmul', out=opv[:, h, :], lhsT=scT[:, h, :], rhs=ih[:, h * 128:(h + 1) * 128], start=False, stop=True,
                          reads=[scT, ih], writes=[ops_], inc=(h == 3))
                nps = pp.get()
                npv = nps[:].rearrange("p (h t) -> p h t", h=4)
                for h in range(4):
                    kb.op('pe', 'matmul', out=npv[:, h, :], lhsT=khat[:, h * 128:(h + 1) * 128], rhs=ih[:, h * 128:(h + 1) * 128], start=True, stop=True,
                          reads=[khat, ih], writes=[nps], inc=(h == 3))
                etot = etots.get()
                kb.op('act', 'activation', out=etot[:], in_=cumS[:, :, last], func=AF.Exp, reads=[cumS], writes=[etot])
                for h in range(4):
                    kb.op('dve', 'scalar_tensor_tensor', out=S[:, h, :], in0=S[:, h, :], scalar=etot[:, h:h + 1], in1=npv[:, h, :],
                          op0=ALU.mult, op1=ALU.add, reads=[S, etot, nps], writes=[S])
                kb.op('act', 'copy', out=Sb[:], in_=S[:], reads=[S], writes=[Sb])
                if dr == 0:
                    osb = osbs.get()
                    kb.op('act', 'copy', out=osb[:], in_=opv, reads=[ops_], writes=[osb])
                    kb.dma('pool', self.ofw.ap()[tok, :], osb[:].rearrange("p h t -> p (h t)"), reads=[osb])
                else:
                    osb = osbs.get()
                    kb.op('dve', 'tensor_tensor', out=osb[:], in0=opv, in1=ofw[:].rearrange("p (h t) -> p h t", h=4), op=ALU.add,
                          reads=[ops_, ofw], writes=[osb])
                    ss = sss.get()
                    sq = sqs.get()
                    for h in range(4):
                        kb.op('act', 'activation', out=sq[:], in_=osb[:, h, :], func=AF.Square, accum_out=ss[:, h:h + 1],
                              reads=[osb], writes=[sq, ss])
                    kb.op('act', 'activation', out=ss[:], in_=ss[:], func=AF.Sqrt, bias=LN_EPS, scale=1.0 / 128, reads=[ss], writes=[ss])
                    kb.op('dve', 'reciprocal', out=ss[:], in_=ss[:], reads=[ss], writes=[ss])
                    hg = hgs.get()
                    kb.op('dve', 'tensor_tensor', out=hg[:], in0=osb[:], in1=ss[:].unsqueeze(2).to_broadcast([128, 4, 128]), op=ALU.mult,
                          reads=[osb, ss], writes=[hg])
                    hgf = hg[:].rearrange("p h t -> p (h t)")
                    kb.op('dve', 'tensor_tensor', out=hgf, in0=hgf, in1=NG[:], op=ALU.mult, reads=[hg, NG], writes=[hg])
                    hgb = hgbs.get()
                    kb.op('dve', 'tensor_tensor', out=hgb[:], in0=hgf, in1=sgh[:], op=ALU.mult, reads=[hg, sgh], writes=[hgb])
                    tps = pp.get()
                    tpv = tps[:].bitcast(BF16).rearrange("p (a b) -> p a b", b=128)
                    for h in range(4):
                        kb.op('pe', 'transpose', out=tpv[:, h, :], in_=hgb[:, h * 128:(h + 1) * 128], identity=self.idb[:],
                              reads=[hgb, self.idb], writes=[tps], inc=(h == 3))
                    hgT = hgTs.get()
                    kb.op('act', 'copy', out=hgT[:], in_=tpv[:, 0:4, :], reads=[tps], writes=[hgT])
                    kb.dma('pool', self.hgT.ap()[:, tok].rearrange("(h k) t -> k h t", k=128), hgT[:], reads=[hgT])
            kb.stage_end()

    def stageC(self, l):
        kb = self.kb
        with ExitStack() as st:
            sb = lambda shape, dt, name: kb.sb(shape, dt, name, stack=st)
            P2 = lambda f, n=2: Pool([f() for _ in range(n)])
            kTd = sb([128, 2, NTOK], BF16, 'kTd')
            for half in range(2):
                kb.dma('sp', kTd[half * 64:(half + 1) * 64, :, :], self.kT.ap().rearrange("(g d) t -> d g t", d=64), writes=[kTd])
            vaug = sb([128, NT, 2, 65], BF16, 'vaug')
            kb.op('dve', 'memset', ap=vaug[:, :, :, 64:65], constant=1.0, writes=[vaug])
            for i0 in range(0, NT, 11):
                for g in range(2):
                    kb.dma('sp', vaug[:, i0:i0 + 11, g, 0:64],
                           self.va.ap()[i0 * 128:(i0 + 11) * 128, g * 64:(g + 1) * 64].rearrange("(i s) d -> s i d", s=128), writes=[vaug])
            snk = sb([128, 8], F32, 'snk')
            kb.dma('sp', snk[:], self.attn_sink.ap()[l].partition_broadcast(128), writes=[snk])
            kb.op('act', 'activation', out=snk[:], in_=snk[:], func=AF.Exp, reads=[snk], writes=[snk])
            qTs = P2(lambda: sb([128, 4, 128], BF16, 'qTc'))
            es = P2(lambda: sb([128, 5, 128], BF16, 'esb'), 3)
            dens = P2(lambda: sb([128, 8], F32, 'den'))
            ats = P2(lambda: sb([128, 8, 64], BF16, 'att'))
            aTs = P2(lambda: sb([128, 4, 128], BF16, 'aT'))
            stp = Pool([kb.ps([128, 2, 512], F32, 'stp', stack=st) for _ in range(2)])
            ops = Pool([kb.ps([128, 2, 512], F32, 'opc', stack=st) for _ in range(1)])
            tpp = Pool([kb.ps([128, 512], F32, 'tpc', stack=st) for _ in range(2)])
            for ti in range(NT):
                tok = slice(ti * 128, (ti + 1) * 128)
                qT = qTs.get()
                kb.dma('sp', qT[:], self.qT.ap()[:, tok].rearrange("(j p) t -> p j t", p=128), writes=[qT])
                if ti < 2:
                    blocks = [(0, None), (1, None)]
                else:
                    blocks = [(0, None), (1, None)]
                    if ti - 1 >= 2:
                        blocks.append((ti - 1, self.ngp))
                    blocks.append((ti, None))
                    if ti + 1 < NT:
                        blocks.append((ti + 1, self.ngn))
                nb = len(blocks)
                op_ = ops.get()
                for h in range(8):
                    g, j, half = h // 4, h // 2, h % 2
                    ps = slice(half * 64, (half + 1) * 64)
                    sp_ = stp.get()
                    spv = sp_[:].rearrange("p a b -> p (a b)").rearrange("p (k t) -> p k t", t=128)
                    for bi, (kt, mk) in enumerate(blocks):
                        kb.op('pe', 'matmul', out=spv[:, bi, :], lhsT=kTd[ps, g, kt * 128:(kt + 1) * 128], rhs=qT[ps, j, :],
                              start=True, stop=(mk is None), reads=[kTd, qT], writes=[sp_], inc=(bi == nb - 1 and mk is None))
                        if mk is not None:
                            kb.op('pe', 'matmul', out=spv[:, bi, :], lhsT=self.idb[:], rhs=mk[:], start=False, stop=True,
                                  reads=[self.idb, mk], writes=[sp_], inc=(bi == nb - 1))
                    e = es.get()
                    kb.op('act', 'activation', out=e[:, 0:nb, :], in_=spv[:, 0:nb, :], func=AF.Exp, scale=0.125, reads=[sp_], writes=[e])
                    oc = (h % 4) * 65
                    for bi, (kt, _) in enumerate(blocks):
                        kb.op('pe', 'matmul', out=op_[:, h // 4, oc:oc + 65], lhsT=e[:, bi, :], rhs=vaug[:, kt, g, :],
                              start=(bi == 0), stop=(bi == nb - 1), reads=[e, vaug], writes=[op_], inc=(bi == nb - 1))
                ov = op_[:, :, 0:260].rearrange("p b (h d) -> p b h d", d=65)
                den = dens.get()
                kb.op('dve', 'tensor_tensor', out=den[:].rearrange("p (b h) -> p b h", b=2), in0=ov[:, :, :, 64],
                      in1=snk[:].rearrange("p (b h) -> p b h", b=2), op=ALU.add, reads=[op_, snk], writes=[den])
                kb.op('dve', 'reciprocal', out=den[:], in_=den[:], reads=[den], writes=[den])
                at = ats.get()
                for b in range(2):
                    kb.op('dve', 'tensor_tensor', out=at[:, b * 4:(b + 1) * 4, :], in0=ov[:, b, :, 0:64],
                          in1=den[:, b * 4:(b + 1) * 4].unsqueeze(2).to_broadcast([128, 4, 64]), op=ALU.mult, reads=[op_, den], writes=[at])
                tp = tpp.get()
                tpv = tp[:].bitcast(BF16).rearrange("p (a b) -> p a b", b=128)
                af = at[:].rearrange("p h d -> p (h d)")
                for j in range(4):
                    kb.op('pe', 'transpose', out=tpv[:, j, :], in_=af[:, j * 128:(j + 1) * 128], identity=self.idb[:],
                          reads=[at, self.idb], writes=[tp], inc=(j == 3))
                aT = aTs.get()
                kb.op('act', 'copy', out=aT[:], in_=tpv[:, 0:4, :], reads=[tp], writes=[aT])
                kb.dma('pool', self.attnT.ap()[:, tok].rearrange("(j p) t -> p j t", p=128), aT[:], reads=[aT])
            kb.stage_end()

    def load_w_bf16(self, st, dram_ap, kc, n, name):
        kb = self.kb
        w = kb.sb([128, kc, n], BF16, name, stack=st)
        if not hasattr(self, '_wst') or self._wst_stage is not st:
            self._wst = Pool([kb.sb([128, 2048], F32, 'wstg', stack=st) for _ in range(2)])
            self._wst_stage = st
            self._wi = 0
        src = dram_ap.rearrange("(kc k) n -> k kc n", k=128)
        step = max(1, 2048 // n)
        for k0 in range(0, kc, step):
            k1 = min(kc, k0 + step)
            ws = self._wst.get()
            wv = ws[:, 0:(k1 - k0) * n].rearrange("p (a b) -> p a b", b=n)
            kb.dma('sp', wv, src[:, k0:k1, :], writes=[ws])
            self._wi += 1
            if self._wi % 2:
                kb.op('dve', 'tensor_copy', out=w[:, k0:k1, :], in_=wv, reads=[ws], writes=[w])
            else:
                kb.op('act', 'copy', out=w[:, k0:k1, :], in_=wv, reads=[ws], writes=[w])
        return w

    def stageD(self, l, xin):
        kb = self.kb
        cp = self.cp
        with ExitStack() as st:
            sb = lambda shape, dt, name: kb.sb(shape, dt, name, stack=st)
            P2 = lambda f, n=2: Pool([f() for _ in range(n)])
            wba = self.load_w_bf16(st, self.w_ba.ap()[l], 4, D, 'wba')
            wbh = self.load_w_bf16(st, self.w_bh.ap()[l], 4, D, 'wbh')
            wo = self.load_w_bf16(st, self.w_out.ap()[l], 8, D, 'wo')
            wr = sb([128, 8, NEXP], F32, 'wr')
            kb.dma('sp', wr[:], self.w_router.ap()[l].rearrange("(kc k) n -> k kc n", k=128), writes=[wr])
            G1 = [self.modtile(st, r, 2, 'G1') for r in range(2)]
            A2 = [self.modtile(st, r, 4, 'A2') for r in range(2)]
            B2 = [self.modtile(st, r, 3, 'B2') for r in range(2)]
            LG = self.bcast_row(st, self.ln_g.ap()[l, 0], D, 'LG')
            LBt = self.bcast_row(st, self.ln_b.ap()[l, 0], D, 'LBt')
            aTs = P2(lambda: sb([128, 4, 512], BF16, 'aTg'), 1)
            hTs = P2(lambda: sb([128, 4, 512], BF16, 'hTg'), 1)
            gas = P2(lambda: sb([128, 8, 512], BF16, 'gag'), 1)
            gbs = P2(lambda: sb([128, 8, 512], BF16, 'gbg'), 1)
            mTs = P2(lambda: sb([128, 8, 512], BF16, 'mT'), 1)
            t1s = P2(lambda: sb([128, 512], F32, 't1'))
            t2s = P2(lambda: sb([128, 512], F32, 't2'))
            xts = P2(lambda: sb([128, D], F32, 'xtd'))
            rrs = P2(lambda: sb([128, D], F32, 'rr'))
            x1s = P2(lambda: sb([128, D], F32, 'x1o'))
            u2s = P2(lambda: sb([128, D], F32, 'u2f'))
            u2bs = P2(lambda: sb([128, D], BF16, 'u2b'))
            u2Ts = P2(lambda: sb([128, 8, 128], F32, 'u2T'))
            stt = P2(lambda: sb([128, 2, 6], F32, 'sttd'), 4)
            mvs = P2(lambda: sb([128, 2], F32, 'mvd'), 4)
            rss = P2(lambda: sb([128, 1], F32, 'rsd'), 4)
            nms = P2(lambda: sb([128, 1], F32, 'nmd'), 4)
            mxs = P2(lambda: sb([128, 1], F32, 'mx'))
            sms = P2(lambda: sb([128, 1], F32, 'sm'))
            exs = P2(lambda: sb([128, NEXP], F32, 'ex'))
            afs = P2(lambda: sb([NEXP, 128], F32, 'af'))
            pa = Pool([kb.ps([128, 512], F32, 'pa', stack=st) for _ in range(2)])
            py = Pool([kb.ps([128, 2, 512], F32, 'py', stack=st) for _ in range(1)])
            pt = Pool([kb.ps([128, 2, 512], F32, 'pt', stack=st) for _ in range(1)])
            pl = Pool([kb.ps([128, 512], F32, 'pl', stack=st) for _ in range(2)])
            for (t0, nt) in groups():
                N = nt * 128
                r = 1 if t0 < 2 else 0
                gtok = slice(t0 * 128, t0 * 128 + N)
                aT, hT, ga, gb, mT = aTs.get(), hTs.get(), gas.get(), gbs.get(), mTs.get()
                kb.dma('sp', aT[:, :, 0:N], self.attnT.ap()[:, gtok].rearrange("(j p) t -> p j t", p=128), writes=[aT])
                kb.dma('sp', hT[:, :, 0:N], self.hgT.ap()[:, gtok].rearrange("(j p) t -> p j t", p=128), writes=[hT])
                kb.dma('sp', ga[:, :, 0:N], self.sgaT.ap()[:, gtok].rearrange("(j p) t -> p j t", p=128), writes=[ga])
                kb.dma('sp', gb[:, :, 0:N], self.sgbT.ap()[:, gtok].rearrange("(j p) t -> p j t", p=128), writes=[gb])
                for n in range(8):
                    pA, pH = pa.get(), pa.get()
                    for kc in range(4):
                        kb.op('pe', 'matmul', out=pA[:, 0:N], lhsT=wba[:, kc, n * 128:(n + 1) * 128], rhs=aT[:, kc, 0:N],
                              start=(kc == 0), stop=(kc == 3), reads=[wba, aT], writes=[pA], inc=(kc == 3))
                    for kc in range(4):
                        kb.op('pe', 'matmul', out=pH[:, 0:N], lhsT=wbh[:, kc, n * 128:(n + 1) * 128], rhs=hT[:, kc, 0:N],
                              start=(kc == 0), stop=(kc == 3), reads=[wbh, hT], writes=[pH], inc=(kc == 3))
                    t1, t2 = t1s.get(), t2s.get()
                    kb.op('dve', 'tensor_tensor', out=t1[:, 0:N], in0=pA[:, 0:N], in1=ga[:, n, 0:N], op=ALU.mult, reads=[pA, ga], writes=[t1])
                    kb.op('dve', 'tensor_tensor', out=t2[:, 0:N], in0=pH[:, 0:N], in1=gb[:, n, 0:N], op=ALU.mult, reads=[pH, gb], writes=[t2])
                    kb.op('dve', 'tensor_tensor', out=mT[:, n, 0:N], in0=t1[:, 0:N], in1=t2[:, 0:N], op=ALU.add, reads=[t1, t2], writes=[mT])
                u2_of = {}
                def part1(j):
                        ti = t0 + j
                        tok = slice(ti * 128, (ti + 1) * 128)
                        xt = xts.get()
                        kb.dma('sp', xt[:], xin.ap()[tok, :], writes=[xt])
                        yp = py.get()
                        for hf in range(2):
                            for kc in range(8):
                                kb.op('pe', 'matmul', out=yp[:, hf, :], lhsT=mT[:, kc, j * 128:(j + 1) * 128], rhs=wo[:, kc, hf * 512:(hf + 1) * 512],
                                      start=(kc == 0), stop=(kc == 7), reads=[mT, wo], writes=[yp], inc=(kc == 7))
                        rr = rrs.get()
                        kb.op('dve', 'tensor_tensor', out=rr[:], in0=yp[:].rearrange("p a b -> p (a b)"), in1=G1[r][:], op=ALU.mult, reads=[yp, G1[r]], writes=[rr])
                        kb.op('dve', 'scalar_tensor_tensor', out=rr[:], in0=xt[:], scalar=ALPHA, in1=rr[:], op0=ALU.mult, op1=ALU.add, reads=[xt, rr], writes=[rr])
                        s6, mv, rs, nm = stt.get(), mvs.get(), rss.get(), nms.get()
                        self.ln_stats(st, rr, rs, nm, s6, mv)
                        x1 = x1s.get()
                        kb.op('act', 'activation', out=x1[:], in_=rr[:], func=AF.Identity, bias=nm[:], scale=rs[:], reads=[rr, nm, rs], writes=[x1])
                        kb.op('dve', 'tensor_tensor', out=x1[:], in0=x1[:], in1=LG[:], op=ALU.mult, reads=[x1, LG], writes=[x1])
                        kb.op('dve', 'tensor_tensor', out=x1[:], in0=x1[:], in1=LBt[:], op=ALU.add, reads=[x1, LBt], writes=[x1])
                        kb.dma('pool', self.x1.ap()[tok, :], x1[:], reads=[x1])
                        s6, mv, rs, nm = stt.get(), mvs.get(), rss.get(), nms.get()
                        self.ln_stats(st, x1, rs, nm, s6, mv)
                        u2 = u2s.get()
                        kb.op('act', 'activation', out=u2[:], in_=x1[:], func=AF.Identity, bias=nm[:], scale=rs[:], reads=[x1, nm, rs], writes=[u2])
                        kb.op('dve', 'tensor_tensor', out=u2[:], in0=u2[:], in1=A2[r][:], op=ALU.mult, reads=[u2, A2[r]], writes=[u2])
                        kb.op('dve', 'tensor_tensor', out=u2[:], in0=u2[:], in1=B2[r][:], op=ALU.add, reads=[u2, B2[r]], writes=[u2])
                        u2b = u2bs.get()
                        kb.op('act', 'copy', out=u2b[:], in_=u2[:], reads=[u2], writes=[u2b])
                        kb.dma('pool', self.u2.ap()[tok, :], u2b[:], reads=[u2b])
                        u2_of[j] = u2
                def part2(j):
                        ti = t0 + j
                        tok = slice(ti * 128, (ti + 1) * 128)
                        u2 = u2_of[j]
                        tp = pt.get()
                        tpv = tp[:].rearrange("p a b -> p (a b)").rearrange("p (k t) -> p k t", t=128)
                        for kc in range(8):
                            kb.op('pe', 'transpose', out=tpv[:, kc, :], in_=u2[:, kc * 128:(kc + 1) * 128], identity=cp[:, C_ID:C_ID + 128],
                                  reads=[u2, cp], writes=[tp], inc=(kc == 7))
                        u2T = u2Ts.get()
                        kb.op('act', 'copy', out=u2T[:], in_=tpv, reads=[tp], writes=[u2T])
                        lp = pl.get()
                        for kc in range(8):
                            kb.op('pe', 'matmul', out=lp[:, 0:NEXP], lhsT=u2T[:, kc, :], rhs=wr[:, kc, :], start=(kc == 0), stop=(kc == 7),
                                  reads=[u2T, wr], writes=[lp], inc=(kc == 7))
                        mx, sm, ex = mxs.get(), sms.get(), exs.get()
                        kb.op('dve', 'tensor_reduce', out=mx[:], in_=lp[:, 0:NEXP], axis=AX.X, op=ALU.max, negate=True, reads=[lp], writes=[mx])
                        kb.op('act', 'activation', out=ex[:], in_=lp[:, 0:NEXP], func=AF.Exp, bias=mx[:], scale=1.0, accum_out=sm[:],
                              reads=[lp, mx], writes=[ex, sm])
                        kb.op('dve', 'reciprocal', out=sm[:], in_=sm[:], reads=[sm], writes=[sm])
                        kb.op('dve', 'tensor_scalar', out=ex[:], in0=ex[:], scalar1=sm[:], scalar2=None, op0=ALU.mult, reads=[ex, sm], writes=[ex])
                        ap_ = pl.get()
                        kb.op('pe', 'transpose', out=ap_[0:NEXP, 0:128], in_=ex[:], identity=cp[:, C_ID:C_ID + 128], reads=[ex, cp], writes=[ap_])
                        af = afs.get()
                        kb.op('act', 'copy', out=af[:], in_=ap_[0:NEXP, 0:128], reads=[ap_], writes=[af])
                        kb.dma('pool', self.affT.ap()[:, tok], af[:], reads=[af])
                part1(0)
                for j in range(nt):
                    if j + 1 < nt:
                        part1(j + 1)
                    part2(j)
            kb.stage_end()

    NROUND = 28

    def stageE(self, l, first):
        kb = self.kb
        cp = self.cp
        NR = self.NROUND
        NC_ = NR * 8
        with ExitStack() as st:
            sb = lambda shape, dt, name: kb.sb(shape, dt, name, stack=st)
            if first:
                zt = sb([128, 2 * (NEXP * LROW + 256) // 128], F32, 'zt')
                kb.op('pool', 'memset', ap=zt[:], constant=0.0, writes=[zt])
                zf = kb.dma('sp', self.pairs.ap().rearrange("(p a) c -> p (a c)", p=128), zt[:], reads=[zt])
            affL = sb([128, 1024], F32, 'affL')
            for e in range(NEXP):
                kb.dma('sp', affL[e * 8:(e + 1) * 8, :], self.affT.ap()[e, LCTX:].rearrange("(s n) -> s n", s=8), writes=[affL])
            affC = sb([NEXP, LCTX], F32, 'affC')
            kb.dma('sp', affC[:], self.affT.ap()[:, 0:LCTX], writes=[affC])
            lo = sb([128, 1], F32, 'lo')
            mid = sb([128, 1], F32, 'mid')
            cnt = sb([128, 1], F32, 'cnt')
            ge = sb([128, 1], F32, 'ge')
            junk = sb([128, 1024], F32, 'junk')
            pp = Pool([kb.ps([128, 512], F32, 'ppE', stack=st) for _ in range(2)])
            kb.op('dve', 'memset', ap=lo[:], constant=0.0, writes=[lo])
            for it in range(28):
                c = 2.0 ** -(it + 1)
                kb.op('dve', 'tensor_scalar_add', out=mid[:], in0=lo[:], scalar1=c, reads=[lo], writes=[mid])
                kb.op('dve', 'tensor_scalar', out=junk[:], in0=affL[:], scalar1=mid[:], scalar2=None, op0=ALU.is_ge, op1=ALU.add,
                      accum_out=cnt[:], reads=[affL, mid], writes=[junk, cnt])
                tp = pp.get()
                kb.op('pe', 'matmul', out=tp[:, 0:1], lhsT=cp[:, C_BD:C_BD + 128], rhs=cnt[:], start=True, stop=True, reads=[cp, cnt], writes=[tp])
                kb.op('dve', 'tensor_scalar', out=ge[:], in0=tp[:, 0:1], scalar1=CAP - 0.5, scalar2=c, op0=ALU.is_ge, op1=ALU.mult,
                      reads=[tp], writes=[ge])
                kb.op('dve', 'tensor_tensor', out=lo[:], in0=lo[:], in1=ge[:], op=ALU.add, reads=[lo, ge], writes=[lo])
            kb.op('dve', 'tensor_scalar', out=junk[:], in0=affL[:], scalar1=lo[:], scalar2=None, op0=ALU.is_ge, op1=ALU.add,
                  accum_out=cnt[:], reads=[affL, lo], writes=[junk, cnt])
            tp = pp.get()
            kb.op('pe', 'matmul', out=tp[:, 0:1], lhsT=cp[:, C_OFF:C_OFF + 128], rhs=cnt[:], start=True, stop=True, reads=[cp, cnt], writes=[tp])
            off = sb([128, 1], F32, 'off')
            kb.op('dve', 'tensor_copy', out=off[:], in_=tp[:, 0:1], reads=[tp], writes=[off])
            work = junk
            kb.op('dve', 'tensor_copy', out=work[:], in_=affL[:], reads=[affL], writes=[work])
            cv = sb([128, NC_], F32, 'cv')
            ci = sb([128, NC_], U32, 'ci')
            for r in range(NR):
                rs_ = slice(r * 8, (r + 1) * 8)
                kb.op('dve', 'max', out=cv[:, rs_], in_=work[:], reads=[work], writes=[cv])
                kb.op('dve', 'max_index', out=ci[:, rs_], in_max=cv[:, rs_], in_values=work[:], reads=[cv, work], writes=[ci])
                kb.op('dve', 'match_replace', out=work[:], in_to_replace=cv[:, rs_], in_values=work[:], imm_value=-1.0, reads=[cv, work], writes=[work])
            rI = cp[:, C_IOTA:C_IOTA + NC_]
            v1 = sb([128, NC_], F32, 'v1')
            sl = sb([128, NC_], F32, 'sl')
            v2 = sb([128, NC_], F32, 'v2')
            kb.op('dve', 'tensor_scalar', out=v1[:], in0=rI, scalar1=cnt[:], scalar2=None, op0=ALU.is_lt, reads=[cp, cnt], writes=[v1])
            kb.op('dve', 'tensor_scalar', out=sl[:], in0=rI, scalar1=off[:], scalar2=None, op0=ALU.add, reads=[cp, off], writes=[sl])
            kb.op('dve', 'tensor_scalar', out=v2[:], in0=sl[:], scalar1=CAP - 0.5, scalar2=None, op0=ALU.is_lt, reads=[sl], writes=[v2])
            kb.op('dve', 'tensor_tensor', out=v1[:], in0=v1[:], in1=v2[:], op=ALU.mult, reads=[v1, v2], writes=[v1])
            BIG = 1.0e6
            kb.op('dve', 'tensor_scalar', out=sl[:], in0=sl[:], scalar1=cp[:, C_EB:C_EB + 1], scalar2=-BIG, op0=ALU.add, op1=ALU.add, reads=[sl, cp], writes=[sl])
            kb.op('dve', 'tensor_tensor', out=sl[:], in0=sl[:], in1=v1[:], op=ALU.mult, reads=[sl, v1], writes=[sl])
            kb.op('dve', 'tensor_scalar_add', out=sl[:], in0=sl[:], scalar1=BIG, reads=[sl], writes=[sl])
            dst = sb([128, NC_], U32, 'dst')
            kb.op('dve', 'tensor_copy', out=dst[:], in_=sl[:], reads=[sl], writes=[dst])
            PR = sb([128, NC_, 2], F32, 'PR')
            kb.op('dve', 'tensor_copy', out=PR[:, :, 0], in_=ci[:], reads=[ci], writes=[PR])
            kb.op('dve', 'tensor_scalar', out=PR[:, :, 0], in0=PR[:, :, 0], scalar1=cp[:, C_SEGB:C_SEGB + 1], scalar2=None, op0=ALU.add, reads=[PR, cp], writes=[PR])
            kb.op('dve', 'tensor_copy', out=PR[:, :, 1], in_=cv[:], reads=[cv], writes=[PR])
            ptab = self.pairs.ap()
            if first:
                kb.wait_dma('pool', *zf)
                kb.wait_dma('sp', *zf)
            kb._deps('pool', [PR, dst], [])
            for r in range(NC_):
                def fn(e, r=r):
                    return e.indirect_dma_start(out=ptab, out_offset=bass.IndirectOffsetOnAxis(ap=dst[:, r:r + 1], axis=0),
                                                in_=PR[:, r, :], in_offset=None, bounds_check=self.breg(e, NEXP * LROW - 1), oob_is_err=False)
                kb.dma('pool', None, None, reads=[PR], fn=fn)
            workc = sb([NEXP, LCTX], F32, 'workc')
            kb.op('dve', 'tensor_copy', out=workc[:], in_=affC[:], reads=[affC], writes=[workc])
            cvc = sb([NEXP, CAPC], F32, 'cvc')
            cic = sb([NEXP, CAPC], U32, 'cic')
            for r in range(CAPC // 8):
                rs_ = slice(r * 8, (r + 1) * 8)
                kb.op('dve', 'max', out=cvc[:, rs_], in_=workc[:], reads=[workc], writes=[cvc])
                kb.op('dve', 'max_index', out=cic[:, rs_], in_max=cvc[:, rs_], in_values=workc[:], reads=[cvc, workc], writes=[cic])
                kb.op('dve', 'match_replace', out=workc[:], in_to_replace=cvc[:, rs_], in_values=workc[:], imm_value=-1.0, reads=[cvc, workc], writes=[workc])
            PRc = sb([NEXP, CAPC, 2], F32, 'PRc')
            kb.op('dve', 'tensor_copy', out=PRc[:, :, 0], in_=cic[:], reads=[cic], writes=[PRc])
            kb.op('dve', 'tensor_copy', out=PRc[:, :, 1], in_=cvc[:], reads=[cvc], writes=[PRc])
            kb.dma('sp', self.pairs.ap()[0:NEXP * LROW, :].rearrange("(e s) c -> e s c", s=LROW)[:, CAP:LROW, :], PRc[:], reads=[PRc])
            kb.stage_end()

    def stageF(self, l):
        kb = self.kb
        with ExitStack() as st:
            sb = lambda shape, dt, name: kb.sb(shape, dt, name, stack=st)
            P2 = lambda f, n=2: Pool([f() for _ in range(n)])
            zt = sb([128, 2048], F32, 'ztf')
            kb.op('pool', 'memset', ap=zt[:], constant=0.0, writes=[zt])
            accv = self.acc.ap().rearrange("(p a) c -> p (a c)", p=128)
            zsem = None
            for i in range(0, NT * 1024, 2048):
                w = min(2048, NT * 1024 - i)
                zsem = kb.dma('sp', accv[:, i:i + w], zt[:, 0:w], reads=[zt])
            L = sb([128, NEXP, 8, 2], F32, 'L')
            ptab = self.pairs.ap()[0:NEXP * LROW, :].rearrange("(e s) c -> e s c", s=LROW)
            for e in range(NEXP):
                kb.dma('sp', L[:, e], ptab[e, 0:CAP, :].rearrange("(p c) two -> p c two", c=8), writes=[L])
            Lc = sb([CAPC, NEXP, 2], F32, 'Lc')
            kb.dma('sp', Lc[:], ptab[:, CAP:LROW, :].rearrange("e j two -> j e two"), writes=[Lc])
            idx = sb([128, NEXP, 8], I32, 'idx')
            idxc = sb([CAPC, NEXP], I32, 'idxc')
            kb.op('dve', 'tensor_copy', out=idx[:], in_=L[:, :, :, 0], reads=[L], writes=[idx])
            kb.op('dve', 'tensor_copy', out=idxc[:], in_=Lc[:, :, 0], reads=[Lc], writes=[idxc])
            wgs = P2(lambda: sb([128, 8, D], BF16, 'wg'))
            wus = P2(lambda: sb([128, 8, D], BF16, 'wu'))
            wds = P2(lambda: sb([128, 8, D], BF16, 'wd'))
            wst = Pool([sb([128, 2048], F32, 'wstf') for _ in range(3)])
            xss = P2(lambda: sb([128, D], BF16, 'xs'), 3)
            xsTs = P2(lambda: sb([128, 8, 512], BF16, 'xsT'))
            hTs = P2(lambda: sb([128, 8, 512], BF16, 'hTe'), 1)
            sgs = P2(lambda: sb([128, 512], BF16, 'sge'))
            ysbs = P2(lambda: sb([128, D], F32, 'ysb'), 3)
            pg = Pool([kb.ps([128, 512], F32, 'pg', stack=st) for _ in range(4)])
            py = Pool([kb.ps([128, 2, 512], F32, 'pye', stack=st) for _ in range(2)])
            u2d = self.u2.ap()
            accd = self.acc.ap()
            wi = 0
            prev_scatter = [zsem] if zsem else []
            def load_pieces(e):
                ws3 = []
                for (src, pool_) in ((self.w_gate, wgs), (self.w_up, wus), (self.w_down, wds)):
                    w = pool_.get()
                    ws3.append(w)
                    sv = src.ap()[l, e].rearrange("(kc k) n -> k kc n", k=128)
                    for k0 in range(0, 8, 2):
                        ws = wst.get()
                        wv = ws[:].rearrange("p (a b) -> p a b", b=D)
                        kb.dma('sp', wv, sv[:, k0:k0 + 2, :], writes=[ws])
                        self._fwi += 1
                        if self._fwi % 2 == 0:
                            kb.op('act', 'copy', out=w[:, k0:k0 + 2, :], in_=wv, reads=[ws], writes=[w])
                        else:
                            kb.op('dve', 'tensor_copy', out=w[:, k0:k0 + 2, :], in_=wv, reads=[ws], writes=[w])
                        yield None
                yield ws3

            def drain(gen):
                r = None
                for r in gen:
                    pass
                return r

            self._fwi = 0
            cur_w = drain(load_pieces(0))
            for e in range(NEXP):
                wg, wu, wd = cur_w
                nxt = load_pieces(e + 1) if e + 1 < NEXP else iter(())
                nxt_w = None
                cur_scatter = []
                for (c0, nch, rows) in ((0, 4, 128), (4, 4, 128), (8, 1, CAPC)):
                    N = nch * rows
                    xsT = xsTs.get()
                    for cc in range(nch):
                        c = c0 + cc
                        xs = xss.get()
                        ia = idx[:, e, c:c + 1] if c < 8 else idxc[:, e:e + 1]
                        irb = idx if c < 8 else idxc
                        def fng(eng, xs=xs, ia=ia, rows=rows):
                            return eng.indirect_dma_start(out=xs[0:rows, :], out_offset=None, in_=u2d,
                                                          in_offset=bass.IndirectOffsetOnAxis(ap=ia, axis=0),
                                                          bounds_check=self.breg(eng, NTOK - 1), oob_is_err=False)
                        kb._deps('pool', [irb], [])
                        kb.dma('pool', None, None, writes=[xs], fn=fng)
                        tp = pg.get()
                        tpv = tp[:].bitcast(BF16).rearrange("p (a b) -> p a b", b=128)
                        for kc in range(8):
                            kb.op('pe', 'transpose', out=tpv[:, kc, 0:rows], in_=xs[0:rows, kc * 128:(kc + 1) * 128], identity=self.idb[0:rows, 0:rows],
                                  reads=[xs, self.idb], writes=[tp], inc=(kc == 7))
                        kb.op('act', 'copy', out=xsT[:, :, cc * rows:(cc + 1) * rows], in_=tpv[:, 0:8, 0:rows], reads=[tp], writes=[xsT])
                    hT = hTs.get()
                    for f in range(8):
                        pG, pU = pg.get(), pg.get()
                        for kc in range(8):
                            kb.op('pe', 'matmul', out=pG[:, 0:N], lhsT=wg[:, kc, f * 128:(f + 1) * 128], rhs=xsT[:, kc, 0:N],
                                  start=(kc == 0), stop=(kc == 7), reads=[wg, xsT], writes=[pG], inc=(kc == 7))
                        for kc in range(8):
                            kb.op('pe', 'matmul', out=pU[:, 0:N], lhsT=wu[:, kc, f * 128:(f + 1) * 128], rhs=xsT[:, kc, 0:N],
                                  start=(kc == 0), stop=(kc == 7), reads=[wu, xsT], writes=[pU], inc=(kc == 7))
                        sg = sgs.get()
                        kb.op('act', 'activation', out=sg[:, 0:N], in_=pG[:, 0:N], func=AF.Silu, reads=[pG], writes=[sg])
                        kb.op('dve', 'tensor_tensor', out=hT[:, f, 0:N], in0=pU[:, 0:N], in1=sg[:, 0:N], op=ALU.mult, reads=[pU, sg], writes=[hT])
                        r_ = next(nxt, None)
                        if r_ is not None:
                            nxt_w = r_
                    for cc in range(nch):
                        c = c0 + cc
                        yp = py.get()
                        for hf in range(2):
                            for f in range(8):
                                kb.op('pe', 'matmul', out=yp[0:rows, hf, :], lhsT=hT[:, f, cc * rows:(cc + 1) * rows], rhs=wd[:, f, hf * 512:(hf + 1) * 512],
                                      start=(f == 0), stop=(f == 7), reads=[hT, wd], writes=[yp], inc=(f == 7))
                        ysb = ysbs.get()
                        wa = L[:, e, c, 1:2] if c < 8 else Lc[:, e, 1:2]
                        wb_ = L if c < 8 else Lc
                        kb.op('dve', 'tensor_scalar', out=ysb[0:rows, :], in0=yp[0:rows].rearrange("p a b -> p (a b)"), scalar1=wa, scalar2=None,
                              op0=ALU.mult, reads=[yp, wb_], writes=[ysb])
                        ia = idx[:, e, c:c + 1] if c < 8 else idxc[:, e:e + 1]
                        def fns(eng, ysb=ysb, ia=ia, rows=rows):
                            return eng.indirect_dma_start(out=accd, out_offset=bass.IndirectOffsetOnAxis(ap=ia, axis=0),
                                                          in_=ysb[0:rows, :], in_offset=None, bounds_check=self.breg(eng, NTOK - 1), oob_is_err=True,
                                                          compute_op=ALU.add)
                        for (s_, v_) in prev_scatter:
                            kb.wait_dma('pool', s_, v_)
                        cur_scatter.append(kb.dma('pool', None, None, reads=[ysb], fn=fns))
                for r_ in nxt:
                    if r_ is not None:
                        nxt_w = r_
                cur_w = nxt_w
                prev_scatter = cur_scatter
            kb.stage_end()

    def stageG(self, l, xout, lat_only):
        kb = self.kb
        with ExitStack() as st:
            sb = lambda shape, dt, name: kb.sb(shape, dt, name, stack=st)
            P2 = lambda f, n=2: Pool([f() for _ in range(n)])
            G2 = [self.modtile(st, r, 5, 'G2') for r in range(2)]
            LG = self.bcast_row(st, self.ln_g.ap()[l, 1], D, 'LG2')
            LBt = self.bcast_row(st, self.ln_b.ap()[l, 1], D, 'LB2')
            x1s = P2(lambda: sb([128, D], F32, 'x1g'), 3)
            acs = P2(lambda: sb([128, D], F32, 'acg'), 3)
            ots = P2(lambda: sb([128, D], F32, 'otg'), 3)
            stt = P2(lambda: sb([128, 2, 6], F32, 'sttg'))
            mvs = P2(lambda: sb([128, 2], F32, 'mvg'))
            rss = P2(lambda: sb([128, 1], F32, 'rsg'))
            nms = P2(lambda: sb([128, 1], F32, 'nmg'))
            for ti in range(2 if lat_only else 0, NT):
                r = 1 if ti < 2 else 0
                tok = slice(ti * 128, (ti + 1) * 128)
                x1, ac = x1s.get(), acs.get()
                kb.dma('sp', x1[:], self.x1.ap()[tok, :], writes=[x1])
                kb.dma('sp', ac[:], self.acc.ap()[tok, :], writes=[ac])
                kb.op('dve', 'tensor_tensor', out=ac[:], in0=ac[:], in1=G2[r][:], op=ALU.mult, reads=[ac, G2[r]], writes=[ac])
                kb.op('dve', 'scalar_tensor_tensor', out=ac[:], in0=x1[:], scalar=ALPHA, in1=ac[:], op0=ALU.mult, op1=ALU.add, reads=[x1, ac], writes=[ac])
                s6, mv, rs, nm = stt.get(), mvs.get(), rss.get(), nms.get()
                self.ln_stats(st, ac, rs, nm, s6, mv)
                ot = ots.get()
                kb.op('act', 'activation', out=ot[:], in_=ac[:], func=AF.Identity, bias=nm[:], scale=rs[:], reads=[ac, nm, rs], writes=[ot])
                kb.op('dve', 'tensor_tensor', out=ot[:], in0=ot[:], in1=LG[:], op=ALU.mult, reads=[ot, LG], writes=[ot])
                kb.op('dve', 'tensor_tensor', out=ot[:], in0=ot[:], in1=LBt[:], op=ALU.add, reads=[ot, LBt], writes=[ot])
                if lat_only:
                    kb.dma('pool', xout.ap()[(ti - 2) * 128:(ti - 1) * 128, :], ot[:], reads=[ot])
                else:
                    kb.dma('pool', xout.ap()[tok, :], ot[:], reads=[ot])
            kb.stage_end()

    def layer(self, l, xin, xout, last):
        self.stage0(l)
        self.stageA(l, xin)
        self.stageB(l, 0)
        self.stageB(l, 1)
        self.stageC(l)
        self.stageD(l, xin)
        self.stageE(l, first=(l == 0))
        self.stageF(l)
        self.stageG(l, xout, lat_only=last)


def build_program():
    P = Prog(debug=False)
    P.layer(0, P.x_all, P.xa, False)
    P.layer(1, P.xa, P.y, True)
    return P


_PROG = None


def kernel(x, c, ctx, c_ctx, w_mod, b_mod, w_in, attn_sink, hgrn_lb_fw, hgrn_lb_bw, hgrn_norm_g,
           w_branch_attn, w_branch_hgrn, w_out, w_router, w_gate, w_up, w_down, ln_g, ln_b):
    global _PROG
    f = lambda a: np.ascontiguousarray(np.asarray(a, dtype=np.float32))
    x, c, ctx, c_ctx = f(x), f(c), f(ctx), f(c_ctx)
    shared = dict(w_mod=f(w_mod), b_mod=f(b_mod), w_in=f(w_in), attn_sink=f(attn_sink), hgrn_lb_fw=f(hgrn_lb_fw),
                  hgrn_lb_bw=f(hgrn_lb_bw), hgrn_norm_g=f(hgrn_norm_g), w_branch_attn=f(w_branch_attn),
                  w_branch_hgrn=f(w_branch_hgrn), w_out=f(w_out), w_router=f(w_router), w_gate=f(w_gate), w_up=f(w_up),
                  w_down=f(w_down), ln_g=f(ln_g), ln_b=f(ln_b))
    cp, rope = make_consts()
    shared['cpack'] = cp
    shared['rope'] = rope
    B = x.shape[0]
    in_maps = []
    for b in range(B):
        d = dict(shared)
        d['x_all'] = np.ascontiguousarray(np.concatenate([ctx[b], x[b]], axis=0))
        d['c2'] = np.ascontiguousarray(np.stack([c[b], c_ctx], 0).reshape(16, 128))
        in_maps.append(d)
    if _PROG is None:
        _PROG = build_program()
    res = run_bass_kernel_spmd(_PROG.kb.nc, in_maps, core_ids=list(range(B)))
    return np.stack([np.asarray(r["y"], dtype=np.float32) for r in res.results], axis=0)
```
